# Optimizing a Trainium2 kernel written in Bass

```python
import jax
import jax.numpy as jnp
from jax import lax
import numpy as np

D_MODEL = 1024
BATCH = 2
SEQ = 16384
DEPTH = 4

GRID_W = 64
CTX_LEN = 256
HEAD_DIM = 64
A_HEADS = 6
A_KV_HEADS = 2
WINDOW = 128
B_HEADS = 4
NA_ROWS = 8
NA_COLS = 16
C_HEADS = 6
C_KV_HEADS = 2
MIX_WIDTH = (A_HEADS + B_HEADS + C_HEADS) * HEAD_DIM
Q_BLOCK = 128
ROPE_BASE = 10000.0
N_EXPERTS = 16
N_EXPERT_GROUPS = 4
TOP_K = 2
D_EXPERT = 512
EXPERT_BLOCK = 512
N_MOD = 6
LN_EPS = 1e-6
QK_EPS = 1e-6
NEG_INF = -1e30
DEEPNORM_ALPHA = (2 * DEPTH) ** 0.25
DEEPNORM_BETA = (8 * DEPTH) ** -0.25
PROJ_SIZES = (A_HEADS * HEAD_DIM, A_KV_HEADS * HEAD_DIM, A_KV_HEADS * HEAD_DIM,
              B_HEADS * HEAD_DIM, B_HEADS * HEAD_DIM, B_HEADS * HEAD_DIM,
              C_HEADS * HEAD_DIM, C_KV_HEADS * HEAD_DIM, C_KV_HEADS * HEAD_DIM)
PROJ_WIDTH = sum(PROJ_SIZES)
VALUE_SLOTS = (2, 5, 8)

kernel_name = 'hybrid_dit_parallel_heads_grouped_moe'


def layer_norm(x, g, b):
    xf = x.astype(jnp.float32)
    mu = jnp.mean(xf, -1, keepdims=True)
    xc = xf - mu
    var = jnp.mean(xc * xc, -1, keepdims=True)
    return (xc * lax.rsqrt(var + LN_EPS) * g.astype(jnp.float32) + b.astype(jnp.float32)).astype(x.dtype)


def rms_norm(x, g):
    xf = x.astype(jnp.float32)
    y = xf * lax.rsqrt(jnp.mean(xf * xf, -1, keepdims=True) + QK_EPS) * g.astype(jnp.float32)
    return y.astype(x.dtype)


def axial_rope_angles(n_tok):
    t = jnp.arange(n_tok, dtype=jnp.int32)
    row = (t // GRID_W).astype(jnp.float32)
    col = (t % GRID_W).astype(jnp.float32)
    axis_dim = HEAD_DIM // 2
    inv_freq = ROPE_BASE ** (-jnp.arange(0, axis_dim, 2, dtype=jnp.float32) / axis_dim)
    ang = jnp.concatenate([row[:, None] * inv_freq, col[:, None] * inv_freq], -1)
    return jnp.cos(ang), jnp.sin(ang)


def apply_rope(x, cos, sin):
    xf = x.astype(jnp.float32).reshape(*x.shape[:-1], HEAD_DIM // 2, 2)
    x0, x1 = xf[..., 0], xf[..., 1]
    cs = cos[None, :, None, :]
    sn = sin[None, :, None, :]
    out = jnp.stack([x0 * cs - x1 * sn, x0 * sn + x1 * cs], -1)
    return out.reshape(x.shape).astype(x.dtype)


def split_heads(p):
    return p.reshape(*p.shape[:-1], p.shape[-1] // HEAD_DIM, HEAD_DIM)


def neighbourhood_indices(n_tok, rows):
    kh = min(NA_ROWS, rows)
    kw = NA_COLS
    t = jnp.arange(n_tok, dtype=jnp.int32)
    r = t // GRID_W
    cq = t % GRID_W
    r0 = jnp.clip(r - kh // 2, 0, rows - kh)
    c0 = jnp.clip(cq - kw // 2, 0, GRID_W - kw)
    kr = r0[:, None, None] + jnp.arange(kh, dtype=jnp.int32)[None, :, None]
    kc = c0[:, None, None] + jnp.arange(kw, dtype=jnp.int32)[None, None, :]
    idx = (kr * GRID_W + kc).reshape(n_tok, kh * kw)
    rel = ((kr - r[:, None, None] + NA_ROWS - 1) * (2 * NA_COLS - 1)
           + (kc - cq[:, None, None] + NA_COLS - 1)).reshape(n_tok, kh * kw)
    return idx, rel


def dense_attention(q, k, v, sink):
    bsz, n_q, n_heads, _ = q.shape
    n_kv = k.shape[2]
    grp = n_heads // n_kv
    n_k = k.shape[1]
    scale = HEAD_DIM ** -0.5
    qg = q.reshape(bsz, n_q, n_kv, grp, HEAD_DIM)
    s = jnp.einsum('bqhgd,bkhd->bhgqk', qg, k, preferred_element_type=jnp.float32) * scale
    if sink is not None:
        sink_col = jnp.broadcast_to(sink.astype(jnp.float32).reshape(1, n_kv, grp, 1, 1), (bsz, n_kv, grp, n_q, 1))
        s = jnp.concatenate([s, sink_col], -1)
    p = jax.nn.softmax(s, -1)[..., :n_k].astype(v.dtype)
    return jnp.einsum('bhgqk,bkhd->bqhgd', p, v).reshape(bsz, n_q, n_heads * HEAD_DIM)


def window_attention(q, k, v, k_ctx, v_ctx, sink):
    bsz, n, n_heads, _ = q.shape
    n_kv = k.shape[2]
    grp = n_heads // n_kv
    n_ctx = k_ctx.shape[1]
    n_blk = n // Q_BLOCK
    span = Q_BLOCK + 2 * WINDOW
    scale = HEAD_DIM ** -0.5
    pad = ((0, 0), (WINDOW, WINDOW), (0, 0), (0, 0))
    k_pad = jnp.pad(k, pad)
    v_pad = jnp.pad(v, pad)
    q_blk = q.reshape(bsz, n_blk, Q_BLOCK, n_kv, grp, HEAD_DIM).swapaxes(0, 1)
    sink_col = jnp.broadcast_to(sink.astype(jnp.float32).reshape(1, n_kv, grp, 1, 1), (bsz, n_kv, grp, Q_BLOCK, 1))

    def block(args):
        q_i, i = args
        start = i * Q_BLOCK
        k_i = lax.dynamic_slice_in_dim(k_pad, start, span, axis=1)
        v_i = lax.dynamic_slice_in_dim(v_pad, start, span, axis=1)
        q_pos = start + jnp.arange(Q_BLOCK, dtype=jnp.int32)
        k_pos = start - WINDOW + jnp.arange(span, dtype=jnp.int32)
        ok = (jnp.abs(q_pos[:, None] - k_pos[None, :]) <= WINDOW) & (k_pos >= 0)[None, :] & (k_pos < n)[None, :]
        s_loc = jnp.einsum('bqhgd,bkhd->bhgqk', q_i, k_i, preferred_element_type=jnp.float32) * scale
        s_loc = jnp.where(ok, s_loc, NEG_INF)
        s_ctx = jnp.einsum('bqhgd,bchd->bhgqc', q_i, k_ctx, preferred_element_type=jnp.float32) * scale
        p = jax.nn.softmax(jnp.concatenate([s_loc, s_ctx, sink_col], -1), -1).astype(v.dtype)
        o = (jnp.einsum('bhgqk,bkhd->bqhgd', p[..., :span], v_i)
             + jnp.einsum('bhgqc,bchd->bqhgd', p[..., span:span + n_ctx], v_ctx))
        return o.reshape(bsz, Q_BLOCK, n_heads * HEAD_DIM)

    o = lax.map(block, (q_blk, jnp.arange(n_blk, dtype=jnp.int32)))
    return o.swapaxes(0, 1).reshape(bsz, n, n_heads * HEAD_DIM)


def neighbourhood_attention(q, k, v, k_ctx, v_ctx, rpb, idx, rel):
    bsz, n, n_heads, _ = q.shape
    n_blk = n // Q_BLOCK
    n_nb = idx.shape[1]
    scale = HEAD_DIM ** -0.5
    q_blk = q.reshape(bsz, n_blk, Q_BLOCK, n_heads, HEAD_DIM).swapaxes(0, 1)
    idx_blk = idx.reshape(n_blk, Q_BLOCK, n_nb)
    rel_blk = rel.reshape(n_blk, Q_BLOCK, n_nb)
    bias_tab = rpb.astype(jnp.float32).reshape(n_heads, -1)

    def block(args):
        q_i, idx_i, rel_i = args
        k_i = k[:, idx_i]
        v_i = v[:, idx_i]
        s_nb = jnp.einsum('bqhd,bqkhd->bhqk', q_i, k_i, preferred_element_type=jnp.float32) * scale
        s_nb = s_nb + bias_tab[:, rel_i][None]
        s_ctx = jnp.einsum('bqhd,bchd->bhqc', q_i, k_ctx, preferred_element_type=jnp.float32) * scale
        p = jax.nn.softmax(jnp.concatenate([s_nb, s_ctx], -1), -1).astype(v.dtype)
        o = (jnp.einsum('bhqk,bqkhd->bqhd', p[..., :n_nb], v_i)
             + jnp.einsum('bhqc,bchd->bqhd', p[..., n_nb:], v_ctx))
        return o.reshape(bsz, Q_BLOCK, n_heads * HEAD_DIM)

    o = lax.map(block, (q_blk, idx_blk, rel_blk))
    return o.swapaxes(0, 1).reshape(bsz, n, n_heads * HEAD_DIM)


def global_attention(q, k, v, k_ctx, v_ctx):
    bsz, n, n_heads, _ = q.shape
    n_kv = k.shape[2]
    grp = n_heads // n_kv
    n_blk = n // Q_BLOCK
    scale = HEAD_DIM ** -0.5
    k_all = jnp.concatenate([k, k_ctx], 1)
    v_all = jnp.concatenate([v, v_ctx], 1)
    q_blk = q.reshape(bsz, n_blk, Q_BLOCK, n_kv, grp, HEAD_DIM).swapaxes(0, 1)

    def block(q_i):
        s = jnp.einsum('bqhgd,bkhd->bhgqk', q_i, k_all, preferred_element_type=jnp.float32) * scale
        p = jax.nn.softmax(s, -1).astype(v.dtype)
        return jnp.einsum('bhgqk,bkhd->bqhgd', p, v_all).reshape(bsz, Q_BLOCK, n_heads * HEAD_DIM)

    o = lax.map(block, q_blk)
    return o.swapaxes(0, 1).reshape(bsz, n, n_heads * HEAD_DIM)


def token_mixers(u, u_c, w_in, w_out, sink, rpb, q_gain, k_gain, cos, sin, na_idx, na_rel, need_ctx_out):
    cuts = [sum(PROJ_SIZES[:i + 1]) for i in range(len(PROJ_SIZES) - 1)]
    qw, kw, vw, qn, kn, vn, qg, kg, vg = [split_heads(p) for p in jnp.split(u @ w_in, cuts, axis=-1)]
    qw_c, kw_c, vw_c, qn_c, kn_c, vn_c, qg_c, kg_c, vg_c = [split_heads(p) for p in jnp.split(u_c @ w_in, cuts, axis=-1)]
    o_w = window_attention(apply_rope(qw, cos, sin), apply_rope(kw, cos, sin), vw, kw_c, vw_c, sink)
    o_n = neighbourhood_attention(qn, kn, vn, kn_c, vn_c, rpb, na_idx, na_rel)
    qg_c = rms_norm(qg_c, q_gain)
    kg_c = rms_norm(kg_c, k_gain)
    o_g = global_attention(apply_rope(rms_norm(qg, q_gain), cos, sin), apply_rope(rms_norm(kg, k_gain), cos, sin),
                           vg, kg_c, vg_c)
    out = jnp.concatenate([o_w, o_n, o_g], -1) @ w_out
    if not need_ctx_out:
        return out, None
    out_c = jnp.concatenate([dense_attention(qw_c, kw_c, vw_c, sink),
                             dense_attention(qn_c, kn_c, vn_c, None),
                             dense_attention(qg_c, kg_c, vg_c, None)], -1) @ w_out
    return out, out_c


def moe_ffn(h, w_router, router_bias, w1, w3, w2):
    n, d = h.shape
    per_group = N_EXPERTS // N_EXPERT_GROUPS
    affinity = jax.nn.sigmoid(jnp.dot(h, w_router, preferred_element_type=jnp.float32))
    select = (affinity + router_bias.astype(jnp.float32)).reshape(n, N_EXPERT_GROUPS, per_group)
    group = jnp.argmax(lax.top_k(select, TOP_K)[0].sum(-1), -1)
    tok = jnp.arange(n, dtype=jnp.int32)
    _, local = lax.top_k(select[tok, group], TOP_K)
    expert = group[:, None] * per_group + local
    gate = affinity[tok[:, None], expert]
    gate = gate / jnp.sum(gate, -1, keepdims=True)
    n_assign = n * TOP_K
    flat_e = expert.reshape(-1)
    order = jnp.argsort(flat_e)
    sorted_e = flat_e[order]
    counts = jnp.bincount(flat_e, length=N_EXPERTS)
    starts = jnp.cumsum(counts) - counts
    padded = (counts + EXPERT_BLOCK - 1) // EXPERT_BLOCK * EXPERT_BLOCK
    pad_ends = jnp.cumsum(padded)
    dest = (pad_ends - padded)[sorted_e] + jnp.arange(n_assign, dtype=jnp.int32) - starts[sorted_e]
    n_blocks = -(-n_assign // EXPERT_BLOCK) + N_EXPERTS
    n_slots = n_blocks * EXPERT_BLOCK
    slot_tok = jnp.full((n_slots,), n, jnp.int32).at[dest].set((order // TOP_K).astype(jnp.int32))
    slot_gate = jnp.zeros((n_slots,), jnp.float32).at[dest].set(gate.reshape(-1)[order])
    block_expert = jnp.minimum(
        jnp.searchsorted(pad_ends, jnp.arange(n_blocks, dtype=jnp.int32) * EXPERT_BLOCK, side='right'),
        N_EXPERTS - 1)
    h_pad = jnp.concatenate([h, jnp.zeros((1, d), h.dtype)], 0)

    def expert_block(args):
        tok_b, gate_b, e = args
        xb = h_pad[tok_b]
        hid = jax.nn.silu(xb @ w1[e]) * (xb @ w3[e])
        return (hid @ w2[e]) * gate_b[:, None].astype(h.dtype)

    y = lax.map(expert_block, (slot_tok.reshape(n_blocks, EXPERT_BLOCK),
                               slot_gate.reshape(n_blocks, EXPERT_BLOCK), block_expert))
    return jnp.zeros((n + 1, d), h.dtype).at[slot_tok].add(y.reshape(n_slots, d))[:n]


def setup_inputs(seed: int = 0) -> dict:
    key = jax.random.key(seed)
    ks = jax.random.split(key, 21)
    nrm = jax.random.normal
    f32 = jnp.float32
    col_scale = np.concatenate([np.full((s,), DEEPNORM_BETA if i in VALUE_SLOTS else 1.0, np.float32)
                                for i, s in enumerate(PROJ_SIZES)])
    return {
        'x': nrm(ks[0], (BATCH, SEQ, D_MODEL), f32),
        'c': nrm(ks[1], (BATCH, D_MODEL), f32),
        'ctx': nrm(ks[2], (BATCH, CTX_LEN, D_MODEL), f32),
        'c_ctx': nrm(ks[3], (D_MODEL,), f32),
        'w_in': nrm(ks[4], (DEPTH, D_MODEL, PROJ_WIDTH), f32) * (D_MODEL ** -0.5) * jnp.asarray(col_scale),
        'w_out': nrm(ks[5], (DEPTH, MIX_WIDTH, D_MODEL), f32) * (MIX_WIDTH ** -0.5 * DEEPNORM_BETA),
        'sink': 0.5 * nrm(ks[6], (DEPTH, A_HEADS), f32),
        'rpb': 0.1 * nrm(ks[7], (DEPTH, B_HEADS, 2 * NA_ROWS - 1, 2 * NA_COLS - 1), f32),
        'q_gain': 1.0 + 0.05 * nrm(ks[8], (DEPTH, HEAD_DIM), f32),
        'k_gain': 1.0 + 0.05 * nrm(ks[9], (DEPTH, HEAD_DIM), f32),
        'w_ada': nrm(ks[10], (DEPTH, D_MODEL, N_MOD * D_MODEL), f32) * (0.1 * D_MODEL ** -0.5),
        'b_ada': 0.01 * nrm(ks[11], (DEPTH, N_MOD * D_MODEL), f32),
        'ln1_g': 1.0 + 0.05 * nrm(ks[12], (DEPTH, D_MODEL), f32),
        'ln1_b': 0.01 * nrm(ks[13], (DEPTH, D_MODEL), f32),
        'ln2_g': 1.0 + 0.05 * nrm(ks[14], (DEPTH, D_MODEL), f32),
        'ln2_b': 0.01 * nrm(ks[15], (DEPTH, D_MODEL), f32),
        'w_router': nrm(ks[16], (D_MODEL, N_EXPERTS), f32) * (D_MODEL ** -0.5),
        'router_bias': 0.01 * nrm(ks[17], (N_EXPERTS,), f32),
        'w1': nrm(ks[18], (DEPTH, N_EXPERTS, D_MODEL, D_EXPERT), f32) * (D_MODEL ** -0.5),
        'w3': nrm(ks[19], (DEPTH, N_EXPERTS, D_MODEL, D_EXPERT), f32) * (D_MODEL ** -0.5),
        'w2': nrm(ks[20], (DEPTH, N_EXPERTS, D_EXPERT, D_MODEL), f32) * (D_EXPERT ** -0.5 * DEEPNORM_BETA),
    }


def reference(x, c, ctx, c_ctx, w_in, w_out, sink, rpb, q_gain, k_gain, w_ada, b_ada,
              ln1_g, ln1_b, ln2_g, ln2_b, w_router, router_bias, w1, w3, w2):
    bsz, n_tok, d = x.shape
    n_ctx = ctx.shape[1]
    rows = n_tok // GRID_W
    cos, sin = axial_rope_angles(n_tok)
    na_idx, na_rel = neighbourhood_indices(n_tok, rows)
    c_act = jax.nn.silu(c)
    cc_act = jax.nn.silu(c_ctx)
    h_ctx = ctx
    for layer in range(DEPTH):
        last = layer == DEPTH - 1
        mod = jnp.split(c_act @ w_ada[layer] + b_ada[layer], N_MOD, axis=-1)
        mod_c = jnp.split(cc_act @ w_ada[layer] + b_ada[layer], N_MOD, axis=-1)
        shift1, scale1, gate1, shift2, scale2, gate2 = [m[:, None, :] for m in mod]
        cshift1, cscale1, cgate1, cshift2, cscale2, cgate2 = mod_c
        u = x * (1 + scale1) + shift1
        u_c = h_ctx * (1 + cscale1) + cshift1
        o, o_c = token_mixers(u, u_c, w_in[layer], w_out[layer], sink[layer], rpb[layer],
                              q_gain[layer], k_gain[layer], cos, sin, na_idx, na_rel, not last)
        x = layer_norm(DEEPNORM_ALPHA * x + (1 + gate1) * o, ln1_g[layer], ln1_b[layer])
        u = x * (1 + scale2) + shift2
        if last:
            f = moe_ffn(u.reshape(-1, d), w_router, router_bias, w1[layer], w3[layer], w2[layer]).reshape(bsz, n_tok, d)
        else:
            h_ctx = layer_norm(DEEPNORM_ALPHA * h_ctx + (1 + cgate1) * o_c, ln1_g[layer], ln1_b[layer])
            u_c = h_ctx * (1 + cscale2) + cshift2
            f_all = moe_ffn(jnp.concatenate([u.reshape(-1, d), u_c.reshape(-1, d)], 0),
                            w_router, router_bias, w1[layer], w3[layer], w2[layer])
            f = f_all[:bsz * n_tok].reshape(bsz, n_tok, d)
            f_c = f_all[bsz * n_tok:].reshape(bsz, n_ctx, d)
            h_ctx = layer_norm(DEEPNORM_ALPHA * h_ctx + (1 + cgate2) * f_c, ln2_g[layer], ln2_b[layer])
        x = layer_norm(DEEPNORM_ALPHA * x + (1 + gate2) * f, ln2_g[layer], ln2_b[layer])
    return x
```

```python
import contextlib
import numpy as np
import ml_dtypes
import concourse.bass as bass
import concourse.mybir as mybir
from concourse.bass_utils import run_bass_kernel_spmd

F32 = mybir.dt.float32
BF16 = mybir.dt.bfloat16
ALU = mybir.AluOpType
AF = mybir.ActivationFunctionType
AX = mybir.AxisListType
NPBF = ml_dtypes.bfloat16

ENGS = ("pe", "act", "dve", "pool", "sp")
SEM_ROT = 1 << 20


class Res:
    __slots__ = ("name", "w", "rs_c", "rs_d")

    def __init__(self, name=""):
        self.name = name
        self.w = None
        self.rs_c = {}
        self.rs_d = []


class Op:
    __slots__ = ("eng", "idx", "fn", "deps", "dma", "signal", "sem", "val", "inc")

    def __init__(self, eng, idx, fn):
        self.eng = eng
        self.idx = idx
        self.fn = fn
        self.deps = ()
        self.dma = None
        self.signal = False
        self.sem = None
        self.val = 0
        self.inc = 16


class Prog:
    def __init__(self, nc):
        self.nc = nc
        self.ops = {e: [] for e in ENGS}
        self.streams = {}

    def res(self, name=""):
        return Res(name)

    def add(self, eng, fn, reads=(), writes=(), dma=None, inc=16):
        op = Op(eng, len(self.ops[eng]), fn)
        op.inc = inc
        deps = {}
        for r in reads:
            if r.w is not None:
                deps[id(r.w)] = r.w
        for w in writes:
            if w.w is not None:
                deps[id(w.w)] = w.w
            for o in w.rs_c.values():
                deps[id(o)] = o
            for o in w.rs_d:
                deps[id(o)] = o
        if dma is not None:
            st = self.streams.setdefault(dma, {"n": 0, "last": None})
            if st["last"] is not None:
                deps[id(st["last"])] = st["last"]
            st["n"] += 1
            op.dma = (dma, st["n"])
            st["last"] = op
        op.deps = tuple(deps.values())
        for r in reads:
            if op.dma is not None:
                r.rs_d.append(op)
            else:
                r.rs_c[eng] = op
        for w in writes:
            w.w = op
            w.rs_c = {}
            w.rs_d = []
        self.ops[eng].append(op)
        return op

    def barrier(self):
        lasts = []
        for e in ENGS:
            comp = [o for o in self.ops[e] if o.dma is None and o.fn is not None]
            if comp:
                lasts.append(comp[-1])
        dmas = [st["last"] for st in self.streams.values() if st["last"] is not None]
        for e in ENGS:
            op = Op(e, len(self.ops[e]), None)
            op.deps = tuple(lasts + dmas)
            self.ops[e].append(op)

    def emit(self, stack):
        nc = self.nc
        for e in ENGS:
            for op in self.ops[e]:
                for d in op.deps:
                    if d.dma is not None:
                        continue
                    if d.eng == op.eng and d.eng == "pe":
                        continue
                    d.signal = True
        nsem = 0
        for e in ENGS:
            cnt = 0
            cur = None
            for op in self.ops[e]:
                if op.dma is not None or not op.signal or op.fn is None:
                    continue
                if cur is None or cnt >= SEM_ROT:
                    cur = stack.enter_context(nc.semaphore(f"s_{e}_{nsem}"))
                    nsem += 1
                    cnt = 0
                cnt += 1
                op.sem = cur
                op.val = cnt
        stream_sems = {}
        for k in self.streams:
            stream_sems[k] = stack.enter_context(nc.semaphore(f"d_{nsem}"))
            nsem += 1
        for e in ENGS:
            for op in self.ops[e]:
                if op.dma is not None:
                    op.sem = stream_sems[op.dma[0]]
                    op.val = op.inc * op.dma[1]
        self.n_sems = nsem

        def run(e, h):
            known = {}
            for op in self.ops[e]:
                waits = {}
                for d in op.deps:
                    if d.dma is None:
                        if d.eng == e and e == "pe":
                            continue
                        if d.sem is None:
                            continue
                    key = id(d.sem)
                    if key not in waits or waits[key][1] < d.val:
                        waits[key] = (d.sem, d.val)
                for key, (sem, val) in waits.items():
                    if known.get(key, 0) >= val:
                        continue
                    known[key] = val
                    h.wait_ge(sem, val)
                if op.fn is None:
                    continue
                ins = op.fn(h)
                if op.dma is not None:
                    ins.then_inc(op.sem, op.inc)
                elif op.signal:
                    ins.then_inc(op.sem, 1)

        with nc.Block() as block:
            @block.tensor
            def _(h):
                run("pe", h)

            @block.scalar
            def _(h):
                run("act", h)

            @block.vector
            def _(h):
                run("dve", h)

            @block.gpsimd
            def _(h):
                run("pool", h)

            @block.sync
            def _(h):
                run("sp", h)


class SbufAlloc:
    def __init__(self, pool_ap, nbytes):
        self.pool = pool_ap
        self.nbytes = nbytes
        self.off = 0
        self.marks = []
        self.peak = 0

    def push(self):
        self.marks.append(self.off)

    def pop(self):
        self.off = self.marks.pop()

    def alloc(self, shape, dtype, parts=128):
        esz = 4 if dtype == F32 else 2
        n = int(np.prod(shape))
        nb = (n * esz + 31) // 32 * 32
        assert self.off + nb <= self.nbytes, f"SBUF overflow {self.off}+{nb}>{self.nbytes}"
        a = self.pool[0:parts, self.off // 4:(self.off + nb) // 4]
        self.off += nb
        self.peak = max(self.peak, self.off)
        if dtype != F32:
            a = a.bitcast(dtype)
        a = a[:, 0:n]
        if len(shape) == 2:
            a = a.rearrange("p (a b) -> p a b", b=shape[1])
        elif len(shape) == 3:
            a = a.rearrange("p (a b c) -> p a b c", b=shape[1], c=shape[2])
        return a


D = 1024
L = 256
HD = 64
GRID_W = 64
NA_ROWS, NA_COLS = 8, 16
NEXP = 16
DEXP = 512
DEPTH = 4
ALPHA = (2 * DEPTH) ** 0.25
LN_EPS = 1e-6
QK_EPS = 1e-6
NEG = -30000.0
SCALE = HD ** -0.5
HALO = 384
SBUF_BYTES = 204 * 1024


class Cfg:
    def __init__(self, S, NQ=4):
        self.S = S
        self.NQ = NQ
        self.T = S // NQ
        self.NT = self.T + L
        self.NQT = self.T // 512
        self.NQB = self.T // 128
        self.EXT = self.T + 2 * HALO
        self.SA = S + L
        self.NKT = self.SA // 128
        self.NBLK = S // 128
        self.NST = self.NT // 128


class Ctx:
    pass


def _bank(K, i, n=512):
    return K.ps[:, i * 512:i * 512 + n]


def _dma(K, out, in_, reads, writes, key, eng="sp"):
    return K.P.add(eng, lambda h: h.dma_start(out=out, in_=in_), reads, writes, dma=key)


def _mm(K, out, lhsT, rhs, start, stop, reads, writes):
    return K.P.add("pe", lambda h: h.matmul(out, lhsT=lhsT, rhs=rhs, start=start, stop=stop), reads, writes)


def _act(K, out, in_, func, reads, writes, scale=1.0, bias=0.0):
    return K.P.add("act", lambda h: h.activation(out=out, in_=in_, func=func, bias=bias, scale=scale), reads, writes)


def _tt(K, out, in0, in1, op, reads, writes, eng="dve"):
    return K.P.add(eng, lambda h: h.tensor_tensor(out=out, in0=in0, in1=in1, op=op), reads, writes)


def _stt(K, out, in0, scalar, in1, op0, op1, reads, writes, eng="dve"):
    return K.P.add(eng, lambda h: h.scalar_tensor_tensor(out=out, in0=in0, scalar=scalar, in1=in1, op0=op0, op1=op1), reads, writes)


def _ts(K, out, in0, s1, s2, op0, op1, reads, writes, eng="dve"):
    if s2 is None:
        return K.P.add(eng, lambda h: h.tensor_scalar(out=out, in0=in0, scalar1=s1, scalar2=None, op0=op0), reads, writes)
    return K.P.add(eng, lambda h: h.tensor_scalar(out=out, in0=in0, scalar1=s1, scalar2=s2, op0=op0, op1=op1), reads, writes)


def _copy(K, out, in_, reads, writes, eng="dve"):
    return K.P.add(eng, lambda h: h.tensor_copy(out=out, in_=in_), reads, writes)


def _bcast_row(K, dram_row_ap, name):
    n = dram_row_ap.shape[-1]
    t = K.A.alloc((n,), F32)
    r = K.P.res(name)
    _dma(K, t, dram_row_ap.broadcast_to([128, n]), [K.r_dram[dram_row_ap.tensor.name]] if dram_row_ap.tensor.name in K.r_dram else [], [r], "bc")
    return t, r


def load_consts(K):
    A, P, d = K.A, K.P, K.d
    K.ident_f = A.alloc((128,), F32)
    K.r_ident_f = P.res()
    _dma(K, K.ident_f, d["ident_f"], [], [K.r_ident_f], "c0")
    K.ident_b = A.alloc((128,), BF16)
    K.r_ident_b = P.res()
    _copy(K, K.ident_b, K.ident_f, [K.r_ident_f], [K.r_ident_b])
    K.ones_f = A.alloc((64,), F32)
    K.r_ones = P.res()
    P.add("dve", lambda h: h.memset(K.ones_f, 1.0), [], [K.r_ones])


def phase_mod(K, w_ada, b_ada2, cond, mod_d):
    A, P = K.A, K.P
    A.push()
    r_mod = K.r_dram[mod_d.tensor.name]
    cond_sb = A.alloc((8, 2), F32)
    r_cond = P.res()
    _dma(K, cond_sb, cond, [], [r_cond], "m0")
    condT = A.alloc((8, 2), F32)
    r_condT = P.res()
    _act(K, condT, cond_sb, AF.Silu, [r_cond], [r_condT])
    bada = A.alloc((6144,), F32, parts=2)
    r_bada = P.res()
    _dma(K, bada, b_ada2, [], [r_bada], "m1")
    modrow = A.alloc((6144,), F32, parts=2)
    r_modrow = P.res()
    wst = [A.alloc((8, 512), F32) for _ in range(2)]
    r_wst = [P.res() for _ in range(2)]
    for cb in range(12):
        s = cb % 2
        _dma(K, wst[s], w_ada[:, cb * 512:(cb + 1) * 512].rearrange("(k p) n -> p k n", p=128), [], [r_wst[s]], f"mw{s}")
        bk = _bank(K, s)
        for k in range(8):
            _mm(K, bk[0:2, :], condT[:, k, :], wst[s][:, k, :], k == 0, k == 7, [r_condT, r_wst[s]], [K.r_ps[s]])
        _tt(K, modrow[:, cb * 512:(cb + 1) * 512], bk[0:2, :], bada[:, cb * 512:(cb + 1) * 512], ALU.add,
            [K.r_ps[s], r_bada], [r_modrow])
    for lo, hi in ((1024, 3072), (4096, 6144)):
        _ts(K, modrow[:, lo:hi], modrow[:, lo:hi], 1.0, None, ALU.add, None, [r_modrow], [r_modrow])
    _dma(K, mod_d, modrow, [r_modrow], [r_mod], "m2")
    P.barrier()
    A.pop()


def phase_qkv(K, x_d, mod_d, w_in_p, gains, ropeC, ropeS, pm_f_d, bd_f_d, qT_d, kdst, vdst):
    A, P, cfg = K.A, K.P, K.cfg
    A.push()
    r_x = K.r_dram[x_d.tensor.name]
    r_q = K.r_dram[qT_d.tensor.name]
    pm_f = A.alloc((128,), F32)
    bd_f = A.alloc((128,), F32)
    r_pm, r_bd = P.res(), P.res()
    _dma(K, pm_f, pm_f_d, [], [r_pm], "q0")
    _dma(K, bd_f, bd_f_d, [], [r_bd], "q1")
    gn = A.alloc((2,), F32)
    r_gn = P.res()
    _dma(K, gn, gains, [], [r_gn], "q2")
    sh_o, r_sh_o = _bcast_row(K, mod_d[0:1, 0:1024], "sh1o")
    sc_o, r_sc_o = _bcast_row(K, mod_d[0:1, 1024:2048], "sc1o")
    sh_c, r_sh_c = _bcast_row(K, mod_d[1:2, 0:1024], "sh1c")
    sc_c, r_sc_c = _bcast_row(K, mod_d[1:2, 1024:2048], "sc1c")
    wbf = A.alloc((8, 2048), BF16)
    r_wbf = P.res()
    wst = [A.alloc((8, 512), F32) for _ in range(2)]
    r_wst = [P.res() for _ in range(2)]
    for cb in range(4):
        s = cb % 2
        _dma(K, wst[s], w_in_p[:, cb * 512:(cb + 1) * 512].rearrange("(k p) n -> p k n", p=128), [], [r_wst[s]], f"qw{s}")
        _act(K, wbf[:, :, cb * 512:(cb + 1) * 512], wst[s], AF.Copy, [r_wst[s]], [r_wbf])
    xs = [A.alloc((1024,), F32) for _ in range(2)]
    r_xs = [P.res() for _ in range(2)]
    tmp = A.alloc((1024,), F32)
    r_tmp = P.res()
    ub = [A.alloc((1024,), BF16) for _ in range(2)]
    r_ub = [P.res() for _ in range(2)]
    uT = [A.alloc((8, 512), BF16) for _ in range(2)]
    r_uT = [P.res() for _ in range(2)]
    rC = [A.alloc((512,), F32) for _ in range(2)]
    rS = [A.alloc((512,), F32) for _ in range(2)]
    r_rope = [P.res() for _ in range(2)]
    qf = [A.alloc((512,), F32) for _ in range(2)]
    r_qf = [P.res() for _ in range(2)]
    sq = A.alloc((512,), F32)
    r_sq = P.res()
    rt = A.alloc((512,), F32)
    r_rt = P.res()
    qn = A.alloc((512,), F32)
    r_qn = P.res()
    t1 = A.alloc((512,), F32)
    r_t1 = P.res()
    t2 = A.alloc((512,), F32)
    r_t2 = P.res()
    ob = [A.alloc((512,), BF16) for _ in range(3)]
    r_ob = [P.res() for _ in range(3)]
    vaug = [A.alloc((8, 65), BF16) for _ in range(2)]
    r_vaug = [P.res() for _ in range(2)]
    for s in range(2):
        P.add("pool", lambda h, s=s: h.memset(vaug[s], 1.0), [], [r_vaug[s]])

    tiles = [(t * 512, 512, False) for t in range(cfg.NQT)] + [(cfg.T, L, True)]
    sub_i = 0
    ob_i = 0
    for ti, (tok0, nt, is_ctx) in enumerate(tiles):
        tp = ti % 2
        sc, sh = (sc_c, sh_c) if is_ctx else (sc_o, sh_o)
        r_sc, r_sh = (r_sc_c, r_sh_c) if is_ctx else (r_sc_o, r_sh_o)
        nsub = nt // 128
        for st in range(nsub):
            s = sub_i % 2
            sub_i += 1
            _dma(K, xs[s], x_d[tok0 + st * 128: tok0 + (st + 1) * 128, :], [r_x], [r_xs[s]], f"qx{s}")
            _tt(K, tmp, xs[s], sc, ALU.mult, [r_xs[s], r_sc], [r_tmp])
            _tt(K, ub[s], tmp, sh, ALU.add, [r_tmp, r_sh], [r_ub[s]])
            bkT = _bank(K, s).bitcast(BF16)
            for k in range(8):
                P.add("pe", lambda h, k=k, s=s, bkT=bkT: h.transpose(out=bkT[:, k * 128:(k + 1) * 128], in_=ub[s][:, k * 128:(k + 1) * 128], identity=K.ident_b),
                      [r_ub[s], K.r_ident_b], [K.r_ps[s]])
            _act(K, uT[tp][:, :, st * 128:(st + 1) * 128], bkT.rearrange("p (a b) -> p a b", b=128), AF.Copy, [K.r_ps[s]], [r_uT[tp]])
        if not is_ctx:
            _dma(K, rC[tp][:, :nt], ropeC[:, tok0:tok0 + nt], [], [r_rope[tp]], f"qr{tp}")
            _dma(K, rS[tp][:, :nt], ropeS[:, tok0:tok0 + nt], [], [r_rope[tp]], f"qs{tp}")
        for ch in range(12):
            kind = ("rope", "rope", "rope", "plain", "plain", "norm", "norm", "norm", "rope", "plain", "plain", "norm")[ch]
            is_k = ch >= 8
            bq = 2 + (ch % 2)
            bkq = _bank(K, bq)
            for k in range(8):
                _mm(K, bkq[:, :nt], wbf[:, k, ch * 128:(ch + 1) * 128], uT[tp][:, k, :nt], k == 0, k == 7, [r_wbf, r_uT[tp]], [K.r_ps[bq]])
            o = ob_i % 3
            ob_i += 1
            if is_k:
                dst = kdst(ch - 8, tok0, nt, is_ctx)
                r_dst = K.r_dram[dst.tensor.name]
            else:
                dst = qT_d[ch][:, tok0:tok0 + nt]
                r_dst = r_q
            if kind == "plain" or (kind == "rope" and is_ctx):
                _act(K, ob[o][:, :nt], bkq[:, :nt], AF.Copy, [K.r_ps[bq]], [r_ob[o]])
            else:
                f = ch % 2
                _act(K, qf[f][:, :nt], bkq[:, :nt], AF.Copy, [K.r_ps[bq]], [r_qf[f]])
                src, r_src = qf[f], r_qf[f]
                bx = 4 + (ch % 2)
                bkx = _bank(K, bx)
                if kind == "norm":
                    _act(K, sq[:, :nt], bkq[:, :nt], AF.Square, [K.r_ps[bq]], [r_sq])
                    _mm(K, bkx[:, :nt], bd_f, sq[:, :nt], True, True, [r_bd, r_sq], [K.r_ps[bx]])
                    _act(K, rt[:, :nt], bkx[:, :nt], AF.Sqrt, [K.r_ps[bx]], [r_rt], scale=1.0 / HD, bias=QK_EPS)
                    P.add("dve", lambda h, nt=nt: h.reciprocal(out=rt[:, :nt], in_=rt[:, :nt]), [r_rt], [r_rt])
                    gcol = gn[:, 1:2] if is_k else gn[:, 0:1]
                    if is_ctx:
                        _stt(K, ob[o][:, :nt], qf[f][:, :nt], gcol, rt[:, :nt], ALU.mult, ALU.mult, [r_qf[f], r_gn, r_rt], [r_ob[o]])
                    else:
                        _stt(K, qn[:, :nt], qf[f][:, :nt], gcol, rt[:, :nt], ALU.mult, ALU.mult, [r_qf[f], r_gn, r_rt], [r_qn])
                        src, r_src = qn, r_qn
                if not is_ctx:
                    bx2 = 6 + (ch % 2)
                    bkx2 = _bank(K, bx2)
                    _mm(K, bkx2[:, :nt], pm_f, src[:, :nt], True, True, [r_pm, r_src], [K.r_ps[bx2]])
                    _tt(K, t1[:, :nt], src[:, :nt], rC[tp][:, :nt], ALU.mult, [r_src, r_rope[tp]], [r_t1])
                    _tt(K, t2[:, :nt], bkx2[:, :nt], rS[tp][:, :nt], ALU.mult, [K.r_ps[bx2], r_rope[tp]], [r_t2])
                    _tt(K, ob[o][:, :nt], t1[:, :nt], t2[:, :nt], ALU.add, [r_t1, r_t2], [r_ob[o]])
            _dma(K, dst, ob[o][:, :nt], [r_ob[o]], [r_dst], f"qo{o}")
        for st in range(nsub):
            s = st % 2
            bv = 4 + s
            bkv = _bank(K, bv)
            for k in range(8):
                _mm(K, bkv, uT[tp][:, k, st * 128:(st + 1) * 128], wbf[:, k, 1536:2048], k == 0, k == 7, [r_uT[tp], r_wbf], [K.r_ps[bv]])
            _act(K, vaug[s][:, :, 0:64], bkv.rearrange("p (a b) -> p a b", b=64), AF.Copy, [K.r_ps[bv]], [r_vaug[s]])
            for (vd, h0, h1) in vdst(tok0 + st * 128, is_ctx):
                _dma(K, vd, vaug[s][:, h0:h1, :].rearrange("p a b -> p (a b)"), [r_vaug[s]], [K.r_dram[vd.tensor.name]], f"qv{s}")
    P.barrier()
    A.pop()


def phase_attn(K, qT_d, kTC_d, VC_d, kTAB_d, VAB_d, kTABc_d, VABc_d, MA_d, MB_d, sink_b, oT_d):
    A, P, cfg = K.A, K.P, K.cfg
    A.push()
    rd = K.r_dram
    r_oT = rd[oT_d.tensor.name]
    rq = [rd[qT_d.tensor.name]]
    r_kvd = [rd[n.tensor.name] for n in (kTC_d, VC_d, kTAB_d, VAB_d, kTABc_d, VABc_d)]
    NKT, NQT, NQB, T = cfg.NKT, cfg.NQT, cfg.NQB, cfg.T

    snk = A.alloc((6,), F32)
    r_snk = P.res()
    _dma(K, snk, sink_b, [], [r_snk], "a0")
    esink = A.alloc((6,), F32)
    r_es = P.res()
    _act(K, esink, snk, AF.Exp, [r_snk], [r_es])

    kTC = A.alloc((cfg.SA,), BF16)
    r_kTC = P.res()
    nchunk = 4
    cw = cfg.SA // nchunk
    for i in range(nchunk):
        lo = i * cw
        hi = cfg.SA if i == nchunk - 1 else (i + 1) * cw
        _dma(K, kTC[:, lo:hi], kTC_d[:, lo:hi], r_kvd, [r_kTC], "a1")
    VC = A.alloc((NKT, 130), BF16)
    r_VC = P.res()
    vch = 13 if NKT % 13 == 0 else NKT
    for i in range(NKT // vch):
        _dma(K, VC[:, i * vch:(i + 1) * vch, :], VC_d[i * vch * 128:(i + 1) * vch * 128, :].rearrange("(t p) c -> p t c", p=128), r_kvd, [r_VC], "a2")
    kTABc = A.alloc((3, L), BF16)
    r_kTABc = P.res()
    _dma(K, kTABc, kTABc_d.rearrange("c p n -> p c n"), r_kvd, [r_kTABc], "a3")
    VABc = A.alloc((2, 390), BF16)
    r_VABc = P.res()
    _dma(K, VABc, VABc_d.rearrange("(t p) c -> p t c", p=128), r_kvd, [r_VABc], "a4")

    qT = [A.alloc((8, 512), BF16) for _ in range(2)]
    r_qT = [P.res() for _ in range(2)]
    kTABw = [A.alloc((3, 1280), BF16) for _ in range(2)]
    VABw = [A.alloc((10, 390), BF16) for _ in range(2)]
    MA = [A.alloc((3, 512), F32) for _ in range(2)]
    r_win = [P.res() for _ in range(2)]
    MBt = [A.alloc((7, 512), F32) for _ in range(2)]
    r_MBt = [P.res() for _ in range(2)]
    Pt = [A.alloc((1024,), BF16) for _ in range(3)]
    r_Pt = [P.res() for _ in range(3)]
    tmpm = [A.alloc((512,), F32) for _ in range(2)]
    r_tmpm = [P.res() for _ in range(2)]
    rs = [A.alloc((512,), F32) for _ in range(2)]
    r_rs = [P.res() for _ in range(2)]
    Bs = [A.alloc((512,), F32) for _ in range(2)]
    r_Bs = [P.res() for _ in range(2)]
    osb = [A.alloc((512,), BF16) for _ in range(2)]
    r_osb = [P.res() for _ in range(2)]
    cnt = {"pt": 0, "nm": 0, "mb": 0, "sb": 0, "ob": 0, "tm": 0}

    pending_norm = []

    def flush_norm():
        while pending_norm:
            pending_norm.pop(0)()

    def normalize(obank, nq, slot, tok0, es_col):
        pending_norm.append(lambda: normalize_now(obank, nq, slot, tok0, es_col))

    def normalize_now(obank, nq, slot, tok0, es_col):
        i = cnt["nm"] % 2
        cnt["nm"] += 1
        ob_ap = _bank(K, obank)
        if es_col is not None:
            _ts(K, rs[i][64:65, :nq], ob_ap[64:65, :nq], esink[64:65, es_col:es_col + 1], None, ALU.add, None, [K.r_ps[obank], r_es], [r_rs[i]])
            P.add("dve", lambda h: h.reciprocal(out=rs[i][64:65, :nq], in_=rs[i][64:65, :nq]), [r_rs[i]], [r_rs[i]])
        else:
            P.add("dve", lambda h: h.reciprocal(out=rs[i][64:65, :nq], in_=ob_ap[64:65, :nq]), [K.r_ps[obank]], [r_rs[i]])
        bb = 6 + i
        bp = _bank(K, bb)
        _mm(K, bp[0:64, :nq], K.ones_f[64:65, 0:64], rs[i][64:65, :nq], True, True, [K.r_ones, r_rs[i]], [K.r_ps[bb]])
        _act(K, Bs[i][0:64, :nq], bp[0:64, :nq], AF.Copy, [K.r_ps[bb]], [r_Bs[i]])
        _tt(K, osb[i][0:64, :nq], ob_ap[0:64, :nq], Bs[i][0:64, :nq], ALU.mult, [K.r_ps[obank], r_Bs[i]], [r_osb[i]])
        _dma(K, oT_d[slot, :, tok0:tok0 + nq], osb[i][0:64, :nq], [r_osb[i]], [r_oT], f"ao{i}")

    def cls_of(lb):
        if lb == 0:
            return 0
        if lb == 1:
            return 1
        if lb == NQB - 2:
            return 3
        if lb == NQB - 1:
            return 4
        return 2

    def attn_ab(tp, t, nq, is_ctx, tok0, c, half, group):
        pb = 64 * half
        if group == "A":
            kc, hv, offs, slot, es_col, moff = 0, half, (-1, 0, 1), 2 * c + half, c + 3 * half, 1
        else:
            hB = 2 * (c - 3) + half
            offs_b = (-3, -2, -1, 0, 1, 2, 3) if (t is None or t == 0 or t == NQT - 1) else (-2, -1, 0, 1, 2)
            kc, hv, offs, slot, es_col, moff = 1 + (c - 3), 2 + hB, offs_b, 2 * c + half, None, 3
        obank = cnt["ob"] % 2
        cnt["ob"] += 1
        ob_ap = _bank(K, obank)
        r_ob = K.r_ps[obank]
        nsteps = 2 + (0 if is_ctx else len(offs))
        step = 0
        if group == "B" and not is_ctx:
            mi = cnt["mb"] % 2
            cnt["mb"] += 1
            for b in range(4):
                _dma(K, MBt[mi][:, :, b * 128:(b + 1) * 128], MB_d[hB, cls_of(4 * t + b)], [], [r_MBt[mi]], f"am{mi}")
        steps = [("ctx", cb) for cb in range(2)]
        if not is_ctx:
            steps += [("win", oi) for oi in range(len(offs))]
        pend = []

        def pv(st_i):
            kind, a, pi = pend[st_i]
            last = st_i == len(steps) - 1
            if kind == "ctx":
                _mm(K, ob_ap[0:65, :nq], VABc[:, a, hv * 65:(hv + 1) * 65], Pt[pi][:, :nq], st_i == 0, last, [r_VABc, r_Pt[pi]], [r_ob])
            else:
                off = offs[a]
                for b in range(4):
                    wb = b + 3 + off
                    _mm(K, ob_ap[0:65, b * 128:(b + 1) * 128], VABw[tp][:, wb, hv * 65:(hv + 1) * 65], Pt[pi][:, b * 128:(b + 1) * 128],
                        False, (last and b == 3), [r_win[tp], r_Pt[pi]], [r_ob])

        for st_i, (kind, a) in enumerate(steps):
            sbk = 2 + (cnt["sb"] % 2) * 2
            cnt["sb"] += 1
            sb_ap = _bank(K, sbk)
            pi = cnt["pt"] % 3
            cnt["pt"] += 1
            if kind == "ctx":
                _mm(K, sb_ap[:, :nq], kTABc[pb:pb + 64, kc, a * 128:(a + 1) * 128], qT[tp][pb:pb + 64, c, :nq], True, True,
                    [r_kTABc, r_qT[tp]], [K.r_ps[sbk]])
                _act(K, Pt[pi][:, :nq], sb_ap[:, :nq], AF.Exp, [K.r_ps[sbk]], [r_Pt[pi]], scale=SCALE)
            else:
                off = offs[a]
                for b in range(4):
                    wb = b + 3 + off
                    _mm(K, sb_ap[:, b * 128:(b + 1) * 128], kTABw[tp][pb:pb + 64, kc, wb * 128:(wb + 1) * 128],
                        qT[tp][pb:pb + 64, c, b * 128:(b + 1) * 128], True, True, [r_win[tp], r_qT[tp]], [K.r_ps[sbk]])
                mi2 = cnt["tm"] % 2
                cnt["tm"] += 1
                if group == "A":
                    m_ap, r_m = MA[tp][:, off + moff, :], r_win[tp]
                else:
                    m_ap, r_m = MBt[mi][:, off + moff, :], r_MBt[mi]
                _stt(K, tmpm[mi2], sb_ap, SCALE, m_ap, ALU.mult, ALU.add, [K.r_ps[sbk], r_m], [r_tmpm[mi2]])
                _act(K, Pt[pi][:, :512], tmpm[mi2], AF.Exp, [r_tmpm[mi2]], [r_Pt[pi]])
            pend.append((kind, a, pi))
            if st_i == 0:
                flush_norm()
            if st_i > 0:
                pv(st_i - 1)
        pv(len(steps) - 1)
        normalize(obank, nq, slot, tok0, es_col)

    def attn_c(tp, nq, is_ctx, tok0, c):
        kts = list(range(NKT - 2, NKT)) if is_ctx else list(range(NKT))
        pis = []

        def pv(i):
            kt = kts[i]
            for half in range(2):
                _mm(K, _bank(K, half)[0:65, :nq], VC[:, kt, half * 65:(half + 1) * 65], Pt[pis[i]][:, half * 512:half * 512 + nq],
                    i == 0, i == len(kts) - 1, [r_VC, r_Pt[pis[i]]], [K.r_ps[half]])

        for i, kt in enumerate(kts):
            sp_ = cnt["sb"] % 2
            cnt["sb"] += 1
            sbk = 2 + sp_ * 2
            for half in range(2):
                pb = 64 * half
                _mm(K, _bank(K, sbk + half)[:, :nq], kTC[pb:pb + 64, kt * 128:(kt + 1) * 128], qT[tp][pb:pb + 64, c, :nq], True, True,
                    [r_kTC, r_qT[tp]], [K.r_ps[sbk + half]])
            pi = cnt["pt"] % 3
            cnt["pt"] += 1
            pis.append(pi)
            s2 = K.ps[:, sbk * 512:(sbk + 2) * 512].rearrange("p (a b) -> p a b", b=512)[:, :, :nq]
            p2 = Pt[pi].rearrange("p (a b) -> p a b", b=512)[:, :, :nq]
            _act(K, p2, s2, AF.Exp, [K.r_ps[sbk], K.r_ps[sbk + 1]], [r_Pt[pi]], scale=SCALE)
            if i == 0:
                flush_norm()
            if i > 0:
                pv(i - 1)
        pv(len(kts) - 1)
        for half in range(2):
            normalize(half, nq, 2 * c + half, tok0, None)

    tiles = [(t, t * 512, 512, False) for t in range(NQT)] + [(None, T, L, True)]
    for ti, (t, tok0, nq, is_ctx) in enumerate(tiles):
        tp = ti % 2
        _dma(K, qT[tp][:, :, :nq], qT_d[:, :, tok0:tok0 + nq].rearrange("c p n -> p c n"), rq, [r_qT[tp]], f"aq{tp}")
        if not is_ctx:
            _dma(K, kTABw[tp], kTAB_d[:, :, t * 512:t * 512 + 1280].rearrange("c p n -> p c n"), r_kvd, [r_win[tp]], f"aw{tp}")
            _dma(K, VABw[tp], VAB_d[t * 512:t * 512 + 1280, :].rearrange("(t p) c -> p t c", p=128), r_kvd, [r_win[tp]], f"av{tp}")
            _dma(K, MA[tp], MA_d[t], [], [r_win[tp]], f"ax{tp}")
        for c in range(3):
            for half in range(2):
                attn_ab(tp, t, nq, is_ctx, tok0, c, half, "A")
        for c in range(3, 5):
            for half in range(2):
                attn_ab(tp, t, nq, is_ctx, tok0, c, half, "B")
        for c in range(5, 8):
            attn_c(tp, nq, is_ctx, tok0, c)
        flush_norm()
    P.barrier()
    A.pop()


def _layer_norm(K, z, r_z, out, r_out, lng, r_lng, lnb, r_lnb, scr):
    P = K.P
    stt, mv, r_s = scr
    for i in range(2):
        P.add("dve", lambda h, i=i: h.bn_stats(out=stt[:, i, :], in_=z[:, i * 512:(i + 1) * 512]), [r_z], [r_s])
    P.add("dve", lambda h: h.bn_aggr(out=mv[:, 0:2], in_=stt.rearrange("p a b -> p (a b)")), [r_s], [r_s])
    _act(K, mv[:, 2:3], mv[:, 1:2], AF.Sqrt, [r_s], [r_s], scale=1.0, bias=LN_EPS)
    P.add("dve", lambda h: h.reciprocal(out=mv[:, 3:4], in_=mv[:, 2:3]), [r_s], [r_s])
    _ts(K, z, z, mv[:, 0:1], mv[:, 3:4], ALU.subtract, ALU.mult, [r_z, r_s], [r_z])
    _tt(K, z, z, lng, ALU.mult, [r_z, r_lng], [r_z])
    _tt(K, out, z, lnb, ALU.add, [r_z, r_lnb], [r_out])


def phase_proj(K, x_d, mod_d, oT_d, w_out_p, lnv, w_router, rbias, x1_d, u2T_d, gT_d):
    A, P, cfg = K.A, K.P, K.cfg
    A.push()
    rd = K.r_dram
    r_x, r_oT, r_x1, r_u2T, r_gT = rd[x_d.tensor.name], rd[oT_d.tensor.name], rd[x1_d.tensor.name], rd[u2T_d.tensor.name], rd[gT_d.tensor.name]
    T = cfg.T
    wo = A.alloc((16, 1024), BF16, parts=64)
    r_wo = P.res()
    wst = [A.alloc((4, 1024), F32, parts=64) for _ in range(2)]
    r_wst = [P.res() for _ in range(2)]
    for g in range(4):
        s = g % 2
        _dma(K, wst[s], w_out_p[g * 4:(g + 1) * 4].rearrange("s p n -> p s n"), [], [r_wst[s]], f"pw{s}")
        _act(K, wo[:, g * 4:(g + 1) * 4, :], wst[s], AF.Copy, [r_wst[s]], [r_wo])
    g1 = [_bcast_row(K, mod_d[i:i + 1, 2048:3072], "g1") for i in range(2)]
    sh2 = [_bcast_row(K, mod_d[i:i + 1, 3072:4096], "sh2") for i in range(2)]
    sc2 = [_bcast_row(K, mod_d[i:i + 1, 4096:5120], "sc2") for i in range(2)]
    lng, r_lng = _bcast_row(K, lnv[0:1, :], "lng1")
    lnb, r_lnb = _bcast_row(K, lnv[1:2, :], "lnb1")
    wr = A.alloc((8, 16), F32)
    r_wr = P.res()
    _dma(K, wr, w_router.rearrange("(k p) e -> p k e", p=128), [], [r_wr], "p0")
    rb, r_rb = _bcast_row(K, rbias, "rb")
    oTt = [A.alloc((16, 128), BF16, parts=64) for _ in range(2)]
    r_oTt = [P.res() for _ in range(2)]
    xs = [A.alloc((1024,), F32) for _ in range(2)]
    r_xs = [P.res() for _ in range(2)]
    tb2 = [A.alloc((1024,), F32) for _ in range(2)]
    r_tb2 = [P.res() for _ in range(2)]
    z2 = [A.alloc((1024,), F32) for _ in range(2)]
    r_z2 = [P.res() for _ in range(2)]
    x1 = [A.alloc((1024,), F32) for _ in range(2)]
    r_x1s = [P.res() for _ in range(2)]
    u22 = [A.alloc((1024,), F32) for _ in range(2)]
    r_u22 = [P.res() for _ in range(2)]
    u2b = [A.alloc((8, 128), BF16) for _ in range(2)]
    r_u2b = [P.res() for _ in range(2)]
    u2f2 = [A.alloc((8, 128), F32) for _ in range(2)]
    r_u2f2 = [P.res() for _ in range(2)]
    stt2 = [A.alloc((2, 6), F32) for _ in range(2)]
    mv2 = [A.alloc((8,), F32) for _ in range(2)]
    r_s2 = [P.res() for _ in range(2)]
    gsc = [[A.alloc((16,), F32) for _ in range(8)] for _ in range(2)]
    r_g2 = [P.res() for _ in range(2)]
    gate = [A.alloc((16,), F32) for _ in range(2)]
    r_gate = [P.res() for _ in range(2)]
    gTs = [A.alloc((128,), F32, parts=16) for _ in range(2)]
    r_gTs = [P.res() for _ in range(2)]

    def v44(a):
        return a.rearrange("p (a b) -> p a b", b=4)

    DBG = getattr(K, "dbg_stop", 99)
    for st in range(cfg.NST if DBG > 0 else 0):
        s = st % 2
        tok = st * 128
        ic = 1 if tok >= T else 0
        tb, r_tb, z, r_z, u2, r_u2, u2f, r_u2f = tb2[s], r_tb2[s], z2[s], r_z2[s], u22[s], r_u22[s], u2f2[s], r_u2f2[s]
        stt, mv, r_s, r_g = stt2[s], mv2[s], r_s2[s], r_g2[s]
        g_aff, g_sel, g_eq, g_s2, g_m, g_ch = gsc[s][0], gsc[s][1], gsc[s][2], gsc[s][3], gsc[s][4], gsc[s][5]
        g_gm, g_dn = gsc[s][6][:, 0:4], gsc[s][7][:, 0:2]
        _dma(K, oTt[s], oT_d[:, :, tok:tok + 128].rearrange("s p n -> p s n"), [r_oT], [r_oTt[s]], f"po{s}")
        _dma(K, xs[s], x_d[tok:tok + 128, :], [r_x], [r_xs[s]], f"px{s}")
        for dh in range(2):
            for sl in range(16):
                _mm(K, _bank(K, dh), oTt[s][:, sl, :], wo[:, sl, dh * 512:(dh + 1) * 512], sl == 0, sl == 15, [r_oTt[s], r_wo], [K.r_ps[dh]])
        _tt(K, tb, K.ps[:, 0:1024], g1[ic][0], ALU.mult, [K.r_ps[0], K.r_ps[1], g1[ic][1]], [r_tb])
        _stt(K, z, xs[s], ALPHA, tb, ALU.mult, ALU.add, [r_xs[s], r_tb], [r_z])
        if DBG <= 1:
            _dma(K, x1_d[tok:tok + 128, :], z, [r_z], [r_x1], f"p1{s}")
            continue
        _layer_norm(K, z, r_z, x1[s], r_x1s[s], lng, r_lng, lnb, r_lnb, (stt, mv, r_s))
        _dma(K, x1_d[tok:tok + 128, :], x1[s], [r_x1s[s]], [r_x1], f"p1{s}")
        if DBG <= 2:
            continue
        _tt(K, u2, x1[s], sc2[ic][0], ALU.mult, [r_x1s[s], sc2[ic][1]], [r_u2])
        _tt(K, u2, u2, sh2[ic][0], ALU.add, [r_u2, sh2[ic][1]], [r_u2])
        for k in range(8):
            bk = 2 + k // 4
            _mm(K, _bank(K, bk)[:, (k % 4) * 128:(k % 4 + 1) * 128], u2[:, k * 128:(k + 1) * 128], K.ident_f, True, True,
                [r_u2, K.r_ident_f], [K.r_ps[bk]])
        tview = K.ps[:, 1024:2048].rearrange("p (a b) -> p a b", b=128)
        if DBG <= 2.3:
            continue
        _act(K, u2f, tview, AF.Copy, [K.r_ps[2], K.r_ps[3]], [r_u2f])
        if DBG <= 2.6:
            continue
        _act(K, u2b[s], u2f, AF.Copy, [r_u2f], [r_u2b[s]])
        if DBG <= 2.8:
            continue
        for k in range(8):
            _dma(K, u2T_d[k, :, tok:tok + 128], u2b[s][:, k, :], [r_u2b[s]], [r_u2T], f"p2{s}")
        if DBG <= 3:
            continue
        lg = _bank(K, 4)[:, 0:16]
        for k in range(8):
            _mm(K, lg, u2f[:, k, :], wr[:, k, :], k == 0, k == 7, [r_u2f, r_wr], [K.r_ps[4]])
        _act(K, g_aff, lg, AF.Sigmoid, [K.r_ps[4]], [r_g])
        _tt(K, g_sel, g_aff, rb, ALU.add, [r_g, r_rb], [r_g])
        P.add("dve", lambda h, g_m=g_m, g_sel=g_sel: h.tensor_reduce(out=g_m[:, 0:4], in_=v44(g_sel), axis=AX.X, op=ALU.max), [r_g], [r_g])
        _tt(K, v44(g_eq), v44(g_sel), g_m[:, 0:4].unsqueeze(2).to_broadcast([128, 4, 4]), ALU.is_equal, [r_g], [r_g])
        _stt(K, g_s2, g_eq, -1e9, g_sel, ALU.mult, ALU.add, [r_g], [r_g])
        P.add("dve", lambda h, g_m=g_m, g_s2=g_s2: h.tensor_reduce(out=g_m[:, 4:8], in_=v44(g_s2), axis=AX.X, op=ALU.max), [r_g], [r_g])
        _tt(K, g_m[:, 8:12], g_m[:, 0:4], g_m[:, 4:8], ALU.add, [r_g], [r_g])
        P.add("dve", lambda h, g_m=g_m: h.tensor_reduce(out=g_m[:, 12:13], in_=g_m[:, 8:12], axis=AX.X, op=ALU.max), [r_g], [r_g])
        _tt(K, g_gm, g_m[:, 8:12], g_m[:, 12:13].to_broadcast([128, 4]), ALU.is_equal, [r_g], [r_g])
        _tt(K, v44(g_ch), v44(g_sel), g_m[:, 4:8].unsqueeze(2).to_broadcast([128, 4, 4]), ALU.is_ge, [r_g], [r_g])
        _tt(K, v44(g_ch), v44(g_ch), g_gm.unsqueeze(2).to_broadcast([128, 4, 4]), ALU.mult, [r_g], [r_g])
        _tt(K, g_ch, g_ch, g_aff, ALU.mult, [r_g], [r_g])
        P.add("dve", lambda h, g_dn=g_dn, g_ch=g_ch: h.tensor_reduce(out=g_dn[:, 0:1], in_=g_ch, axis=AX.X, op=ALU.add), [r_g], [r_g])
        P.add("dve", lambda h, g_dn=g_dn: h.reciprocal(out=g_dn[:, 1:2], in_=g_dn[:, 0:1]), [r_g], [r_g])
        _ts(K, gate[s], g_ch, g_dn[:, 1:2], None, ALU.mult, None, [r_g], [r_gate[s]])
        if DBG <= 4:
            continue
        _mm(K, _bank(K, 5)[0:16, 0:128], gate[s], K.ident_f, True, True, [r_gate[s], K.r_ident_f], [K.r_ps[5]])
        _act(K, gTs[s], _bank(K, 5)[0:16, 0:128], AF.Copy, [K.r_ps[5]], [r_gTs[s]])
        _dma(K, gT_d[:, tok:tok + 128], gTs[s], [r_gTs[s]], [r_gT], f"p3{s}")
    P.barrier()
    A.pop()


def phase_moe(K, x1_d, mod_d, u2T_d, gT_d, w1, w3, w2, lnv, sel_d, xo_d):
    A, P, cfg = K.A, K.P, K.cfg
    A.push()
    rd = K.r_dram
    r_x1, r_u2T, r_gT, r_xo = rd[x1_d.tensor.name], rd[u2T_d.tensor.name], rd[gT_d.tensor.name], rd[xo_d.tensor.name]
    T, NST = cfg.T, cfg.NST
    g2 = [_bcast_row(K, mod_d[i:i + 1, 5120:6144], "g2") for i in range(2)]
    lng, r_lng = _bcast_row(K, lnv[2:3, :], "lng2")
    lnb, r_lnb = _bcast_row(K, lnv[3:4, :], "lnb2")
    sel = A.alloc((16, 128), F32, parts=16)
    r_sel = P.res()
    _dma(K, sel, sel_d.rearrange("k (e m) -> k e m", m=128), [], [r_sel], "e0")
    npass = (NST + 11) // 12
    nsm = (NST + npass - 1) // npass
    halves = [(i * nsm, min(NST, (i + 1) * nsm)) for i in range(npass)]
    uT2 = A.alloc((8, nsm * 128), BF16)
    r_uT2 = P.res()
    gTt = A.alloc((nsm * 128,), F32, parts=16)
    r_gTt = P.res()
    yacc = A.alloc((nsm, 1024), F32)
    r_yacc = P.res()
    w1b = [A.alloc((8, 512), BF16) for _ in range(2)]
    w3b = [A.alloc((8, 512), BF16) for _ in range(2)]
    w2b = [A.alloc((4, 1024), BF16) for _ in range(2)]
    r_wb = [P.res() for _ in range(2)]
    stg = [A.alloc((1024,), F32) for _ in range(2)]
    r_stg = [P.res() for _ in range(2)]
    hidb = [A.alloc((4, 512), BF16) for _ in range(2)]
    r_hidb = [P.res() for _ in range(2)]
    s_sb = [A.alloc((512,), F32) for _ in range(2)]
    r_ssb = [P.res() for _ in range(2)]
    t_sb = [A.alloc((512,), F32) for _ in range(2)]
    r_tsb = [P.res() for _ in range(2)]
    xs = [A.alloc((1024,), F32) for _ in range(2)]
    r_xs = [P.res() for _ in range(2)]
    tb = A.alloc((1024,), F32)
    r_tb = P.res()
    z = A.alloc((1024,), F32)
    r_z = P.res()
    xo = [A.alloc((1024,), F32) for _ in range(2)]
    r_xos = [P.res() for _ in range(2)]
    stt = A.alloc((2, 6), F32)
    mv = A.alloc((8,), F32)
    r_s = P.res()
    cnt = {"stg": 0, "h": 0, "y": 0, "g": 0, "grp": 0, "ex": 0}
    pending_y = []

    for (s_lo, s_hi) in halves:
        nsub = s_hi - s_lo
        if nsub <= 0:
            continue
        tok_lo = s_lo * 128
        ntok = nsub * 128
        _dma(K, uT2[:, :, :ntok], u2T_d[:, :, tok_lo:tok_lo + ntok].rearrange("k p n -> p k n"), [r_u2T], [r_uT2], "e1")
        _dma(K, gTt[:, :ntok], gT_d[:, tok_lo:tok_lo + ntok], [r_gT], [r_gTt], "e2")
        groups = [(g0, min(4, nsub - g0)) for g0 in range(0, nsub, 4)]
        for e in range(NEXP):
            wp = cnt["ex"] % 2
            cnt["ex"] += 1
            pieces = []
            for j in range(4):
                pieces.append((w1[e][j * 256:(j + 1) * 256, :].rearrange("(k p) n -> p k n", p=128), w1b[wp][:, 2 * j:2 * j + 2, :], 2, 512))
            for j in range(4):
                pieces.append((w3[e][j * 256:(j + 1) * 256, :].rearrange("(k p) n -> p k n", p=128), w3b[wp][:, 2 * j:2 * j + 2, :], 2, 512))
            for j in range(4):
                pieces.append((w2[e][j * 128:(j + 1) * 128, :].rearrange("(k p) n -> p k n", p=128), w2b[wp][:, j:j + 1, :], 1, 1024))
            for (src, dst, a, b) in pieces:
                si = cnt["stg"] % 2
                cnt["stg"] += 1
                sv = stg[si].rearrange("p (a b) -> p a b", b=b)
                _dma(K, sv, src, [], [r_stg[si]], f"es{si}")
                _act(K, dst, sv, AF.Copy, [r_stg[si]], [r_wb[wp]])
            for (g0, gn) in groups:
                n = gn * 128
                c0 = g0 * 128
                gbk = 4 + cnt["g"] % 2
                cnt["g"] += 1
                gb = _bank(K, gbk)
                _mm(K, gb[:, :n], sel[0:16, e, :], gTt[0:16, c0:c0 + n], True, True, [r_sel, r_gTt], [K.r_ps[gbk]])
                hp = cnt["grp"] % 2
                cnt["grp"] += 1
                for fc in range(4):
                    hb = cnt["h"] % 2
                    cnt["h"] += 1
                    b1, b3 = hb, 2 + hb
                    for k in range(8):
                        _mm(K, _bank(K, b1)[:, :n], w1b[wp][:, k, fc * 128:(fc + 1) * 128], uT2[:, k, c0:c0 + n], k == 0, k == 7, [r_wb[wp], r_uT2], [K.r_ps[b1]])
                    for k in range(8):
                        _mm(K, _bank(K, b3)[:, :n], w3b[wp][:, k, fc * 128:(fc + 1) * 128], uT2[:, k, c0:c0 + n], k == 0, k == 7, [r_wb[wp], r_uT2], [K.r_ps[b3]])
                    _act(K, s_sb[hb][:, :n], _bank(K, b1)[:, :n], AF.Silu, [K.r_ps[b1]], [r_ssb[hb]])
                    _tt(K, t_sb[hb][:, :n], s_sb[hb][:, :n], _bank(K, b3)[:, :n], ALU.mult, [r_ssb[hb], K.r_ps[b3]], [r_tsb[hb]])
                    _tt(K, hidb[hp][:, fc, :n], t_sb[hb][:, :n], gb[:, :n], ALU.mult, [r_tsb[hb], K.r_ps[gbk]], [r_hidb[hp]])
                def y_part(e=e, g0=g0, gn=gn, hp=hp, wp=wp):
                    for j in range(gn):
                        sub = g0 + j
                        for dh in range(2):
                            yb = 6 + cnt["y"] % 2
                            cnt["y"] += 1
                            for fc in range(4):
                                _mm(K, _bank(K, yb), hidb[hp][:, fc, j * 128:(j + 1) * 128], w2b[wp][:, fc, dh * 512:(dh + 1) * 512], fc == 0, fc == 3,
                                    [r_hidb[hp], r_wb[wp]], [K.r_ps[yb]])
                            ya = yacc[:, sub, dh * 512:(dh + 1) * 512]
                            if e == 0:
                                _copy(K, ya, _bank(K, yb), [K.r_ps[yb]], [r_yacc])
                            else:
                                _tt(K, ya, ya, _bank(K, yb), ALU.add, [r_yacc, K.r_ps[yb]], [r_yacc])
                while pending_y:
                    pending_y.pop(0)()
                pending_y.append(y_part)
        while pending_y:
            pending_y.pop(0)()
        for j in range(nsub):
            st = s_lo + j
            s = st % 2
            tok = st * 128
            ic = 1 if tok >= T else 0
            _dma(K, xs[s], x1_d[tok:tok + 128, :], [r_x1], [r_xs[s]], f"ex{s}")
            _tt(K, tb, yacc[:, j, :], g2[ic][0], ALU.mult, [r_yacc, g2[ic][1]], [r_tb])
            _stt(K, z, xs[s], ALPHA, tb, ALU.mult, ALU.add, [r_xs[s], r_tb], [r_z])
            _layer_norm(K, z, r_z, xo[s], r_xos[s], lng, r_lng, lnb, r_lnb, (stt, mv, r_s))
            _dma(K, xo_d[tok:tok + 128, :], xo[s], [r_xos[s]], [r_xo], f"ey{s}")
    P.barrier()
    A.pop()


def _new_ctx(nc, cfg, st):
    K = Ctx()
    K.nc = nc
    K.cfg = cfg
    pool = st.enter_context(nc.sbuf_tensor("pool", [128, SBUF_BYTES // 4], F32))
    ps = st.enter_context(nc.psum_tensor("ps", [128, 4096], F32))
    K.ps = ps[:]
    K.A = SbufAlloc(pool[:], SBUF_BYTES)
    K.P = Prog(nc)
    K.r_ps = [K.P.res(f"ps{i}") for i in range(8)]
    K.r_dram = {}
    K.d = {}
    return K


def _dt(K, name, shape, dtype, kind):
    t = K.nc.dram_tensor(name, list(shape), dtype, kind=kind).ap()
    K.d[name] = t
    K.r_dram[name] = K.P.res(name)
    return t


def build_fused(cfg, nlayers=DEPTH):
    assert cfg.NQ == 1
    nc = bass.Bass("TRN2", target_bir_lowering=False)
    T, NT, SA, EXT = cfg.T, cfg.NT, cfg.SA, cfg.EXT
    with contextlib.ExitStack() as st:
        K = _new_ctx(nc, cfg, st)
        A, P = K.A, K.P
        I, O, N = "ExternalInput", "ExternalOutput", "Internal"
        x_in = _dt(K, "x_in", [NT, D], F32, I)
        cond = _dt(K, "cond", [128, 8, 2], F32, I)
        w_ada = _dt(K, "w_ada", [DEPTH, D, 6 * D], F32, I)
        b_ada2 = _dt(K, "b_ada2", [DEPTH, 2, 6 * D], F32, I)
        w_in_p = _dt(K, "w_in_p", [DEPTH, D, 2048], F32, I)
        gains = _dt(K, "gains", [DEPTH, 128, 2], F32, I)
        ropeC = _dt(K, "ropeC", [128, T], F32, I)
        ropeS = _dt(K, "ropeS", [128, T], F32, I)
        _dt(K, "ident_f", [128, 128], F32, I)
        pm_f = _dt(K, "pm_f", [128, 128], F32, I)
        bd_f = _dt(K, "bd_f", [128, 128], F32, I)
        MA_d = _dt(K, "MA_d", [cfg.NQT, 128, 3, 512], F32, I)
        MB_d = _dt(K, "MB_d", [DEPTH, 4, 5, 128, 7, 128], F32, I)
        sink_b = _dt(K, "sink_b", [DEPTH, 128, 6], F32, I)
        w_out_p = _dt(K, "w_out_p", [DEPTH, 16, 64, D], F32, I)
        lnv = _dt(K, "lnv", [DEPTH, 4, D], F32, I)
        w_router = _dt(K, "w_router", [D, NEXP], F32, I)
        rbias = _dt(K, "rbias", [1, NEXP], F32, I)
        sel_d = _dt(K, "sel", [16, 16 * 128], F32, I)
        w1 = _dt(K, "w1", [DEPTH, NEXP, D, DEXP], F32, I)
        w3 = _dt(K, "w3", [DEPTH, NEXP, D, DEXP], F32, I)
        w2 = _dt(K, "w2", [DEPTH, NEXP, DEXP, D], F32, I)
        x_out = _dt(K, "x_out", [T, D], F32, O)
        x_d = _dt(K, "x_d", [NT, D], F32, N)
        mod_d = [_dt(K, f"mod_d{l}", [2, 6 * D], F32, N) for l in range(nlayers)]
        qT_d = _dt(K, "qT_d", [8, 128, NT], BF16, N)
        kTC_d = _dt(K, "kTC_d", [128, SA], BF16, N)
        VC_d = _dt(K, "VC_d", [SA, 130], BF16, N)
        kTAB_d = _dt(K, "kTAB_d", [3, 128, EXT], BF16, N)
        VAB_d = _dt(K, "VAB_d", [EXT, 390], BF16, N)
        kTABc_d = _dt(K, "kTABc_d", [3, 128, L], BF16, N)
        VABc_d = _dt(K, "VABc_d", [L, 390], BF16, N)
        oT_d = _dt(K, "oT_d", [16, 64, NT], BF16, N)
        x1_d = _dt(K, "x1_d", [NT, D], F32, N)
        u2T_d = _dt(K, "u2T_d", [8, 128, NT], BF16, N)
        gT_d = _dt(K, "gT_d", [16, NT], F32, N)
        rd = K.r_dram
        load_consts(K)
        nchunk = 8
        rows = NT // nchunk
        for i in range(nchunk):
            lo = i * rows
            hi = NT if i == nchunk - 1 else (i + 1) * rows
            _dma(K, x_d[lo:hi, :], x_in[lo:hi, :], [], [rd["x_d"]], f"i{i % 2}")
        A.push()
        zt = A.alloc((3 * HALO,), BF16)
        r_zt = P.res()
        P.add("dve", lambda h: h.memset(zt, 0.0), [], [r_zt])
        for off in (0, HALO + T):
            _dma(K, kTAB_d[:, :, off:off + HALO].rearrange("c p n -> p c n"), zt.rearrange("p (c n) -> p c n", c=3), [r_zt], [rd["kTAB_d"]], "z0")
            for j in range(HALO // 128):
                _dma(K, VAB_d[off + j * 128:off + (j + 1) * 128, :], zt[:, 0:390], [r_zt], [rd["VAB_d"]], "z1")
        P.barrier()
        A.pop()
        for l in range(nlayers):
            phase_mod(K, w_ada[l], b_ada2[l], cond, mod_d[l])

        def kdst(idx, tok0, nt, is_ctx):
            if idx == 3:
                return kTC_d[:, tok0:tok0 + nt]
            if is_ctx:
                return kTABc_d[idx][:, 0:nt]
            return kTAB_d[idx][:, HALO + tok0:HALO + tok0 + nt]

        def vdst(tok, is_ctx):
            if is_ctx:
                return [(VABc_d[tok - T:tok - T + 128, :], 0, 6), (VC_d[tok:tok + 128, :], 6, 8)]
            return [(VAB_d[HALO + tok:HALO + tok + 128, :], 0, 6), (VC_d[tok:tok + 128, :], 6, 8)]

        for l in range(nlayers):
            phase_qkv(K, x_d, mod_d[l], w_in_p[l], gains[l], ropeC, ropeS, pm_f, bd_f, qT_d, kdst, vdst)
            phase_attn(K, qT_d, kTC_d, VC_d, kTAB_d, VAB_d, kTABc_d, VABc_d, MA_d, MB_d[l], sink_b[l], oT_d)
            phase_proj(K, x_d, mod_d[l], oT_d, w_out_p[l], lnv[l], w_router, rbias, x1_d, u2T_d, gT_d)
            phase_moe(K, x1_d, mod_d[l], u2T_d, gT_d, [w1[l][e] for e in range(NEXP)], [w3[l][e] for e in range(NEXP)],
                      [w2[l][e] for e in range(NEXP)], lnv[l], sel_d, x_d)
        rows = T // nchunk
        for i in range(nchunk):
            _dma(K, x_out[i * rows:(i + 1) * rows, :], x_d[i * rows:(i + 1) * rows, :], [rd["x_d"]], [rd["x_out"]], f"i{i % 2}")
        P.barrier()
        P.emit(st)
        K.stats = (K.A.peak, K.P.n_sems, {e: len(K.P.ops[e]) for e in ENGS})
    return nc, K.stats


def _perm_w_in():
    def cols(base, h):
        return list(range(base + 64 * h, base + 64 * (h + 1)))
    p = []
    for a, b in ((0, 3), (1, 4), (2, 5)):
        p += cols(0, a) + cols(0, b)
    for a, b in ((0, 1), (2, 3)):
        p += cols(640, a) + cols(640, b)
    for a, b in ((0, 3), (1, 4), (2, 5)):
        p += cols(1408, a) + cols(1408, b)
    p += cols(384, 0) + cols(384, 1)
    p += cols(896, 0) + cols(896, 1) + cols(896, 2) + cols(896, 3)
    p += cols(1792, 0) + cols(1792, 1)
    p += cols(512, 0) + cols(512, 1)
    for h in range(4):
        p += cols(1152, h)
    p += cols(1920, 0) + cols(1920, 1)
    return np.array(p)


def _perm_w_out():
    rows = []
    for h in (0, 3, 1, 4, 2, 5):
        rows.append(np.arange(h * 64, (h + 1) * 64))
    for h in range(4):
        rows.append(np.arange(384 + h * 64, 384 + (h + 1) * 64))
    for h in (0, 3, 1, 4, 2, 5):
        rows.append(np.arange(640 + h * 64, 640 + (h + 1) * 64))
    return np.stack(rows)


def _rope_tables(cfg, q):
    t = np.arange(q * cfg.T, (q + 1) * cfg.T)
    row = (t // GRID_W).astype(np.float32)
    col = (t % GRID_W).astype(np.float32)
    axis_dim = HD // 2
    inv_freq = (np.float32(10000.0) ** (-np.arange(0, axis_dim, 2, dtype=np.float32) / np.float32(axis_dim))).astype(np.float32)
    ang = np.concatenate([row[:, None] * inv_freq, col[:, None] * inv_freq], -1).astype(np.float32)
    cos, sin = np.cos(ang).astype(np.float32), np.sin(ang).astype(np.float32)
    p = np.arange(128)
    d = p % 64
    pair = d // 2
    C = cos[:, pair].T.copy()
    sgn = np.where(d % 2 == 0, -1.0, 1.0).astype(np.float32)
    S = (sin[:, pair].T * sgn[:, None]).astype(np.float32)
    return np.ascontiguousarray(C), np.ascontiguousarray(S)


def _mask_A(cfg, q):
    M = np.full((cfg.NQT, 128, 3, 512), NEG, np.float32)
    ar = np.arange(128)
    for t in range(cfg.NQT):
        for b in range(4):
            gi = q * cfg.NQB + 4 * t + b
            qpos = gi * 128 + ar
            for oi, off in enumerate((-1, 0, 1)):
                gj = gi + off
                if gj < 0 or gj >= cfg.NBLK:
                    continue
                kpos = gj * 128 + ar
                ok = np.abs(qpos[None, :] - kpos[:, None]) <= 128
                M[t, :, oi, b * 128:(b + 1) * 128] = np.where(ok, 0.0, NEG)
    return M


def _mask_B(cfg, q, rpb_l):
    rows = cfg.S // GRID_W
    kh, kw = min(NA_ROWS, rows), NA_COLS
    M = np.full((4, 5, 128, 7, 128), NEG, np.float32)
    ar = np.arange(128)
    reps = {0: 0, 1: 1, 2: 2, 3: cfg.NQB - 2, 4: cfg.NQB - 1}
    for cl, lb in reps.items():
        if cl == 2 and cfg.NQB <= 4:
            continue
        gi = q * cfg.NQB + lb
        qpos = gi * 128 + ar
        r = qpos // GRID_W
        cq = qpos % GRID_W
        r0 = np.clip(r - kh // 2, 0, rows - kh)
        c0 = np.clip(cq - kw // 2, 0, GRID_W - kw)
        for oi, off in enumerate(range(-3, 4)):
            gj = gi + off
            if gj < 0 or gj >= cfg.NBLK:
                continue
            kpos = gj * 128 + ar
            kr = (kpos // GRID_W)[:, None]
            kc = (kpos % GRID_W)[:, None]
            ok = (kr >= r0[None]) & (kr < r0[None] + kh) & (kc >= c0[None]) & (kc < c0[None] + kw)
            ri = np.clip(kr - r[None] + NA_ROWS - 1, 0, 2 * NA_ROWS - 2)
            ci = np.clip(kc - cq[None] + NA_COLS - 1, 0, 2 * NA_COLS - 2)
            for h in range(4):
                M[h, cl, :, oi, :] = np.where(ok, rpb_l[h][ri, ci], NEG)
    return M


def _consts():
    ident = np.eye(128, dtype=np.float32)
    p = np.arange(128)
    pm = (p[:, None] == (p[None, :] ^ 1)).astype(np.float32)
    bd = ((p[:, None] // 64) == (p[None, :] // 64)).astype(np.float32)
    sel = np.zeros((16, 16, 128), np.float32)
    for e in range(16):
        sel[e, e, :] = 1.0
    return ident, pm, bd, sel.reshape(16, 16 * 128)


def host_inputs(cfg, inputs):
    ident, pm, bd, sel = _consts()
    perm = _perm_w_in()
    f = lambda a: np.ascontiguousarray(a, dtype=np.float32)
    w_in_p = f(inputs["w_in"][:, :, perm])
    w_out_p = f(inputs["w_out"][:, _perm_w_out()])
    lnv = f(np.stack([inputs["ln1_g"], inputs["ln1_b"], inputs["ln2_g"], inputs["ln2_b"]], 1))
    b_ada2 = f(np.stack([inputs["b_ada"]] * 2, 1))
    gains = f(np.stack([np.tile(inputs["q_gain"], (1, 2)), np.tile(inputs["k_gain"], (1, 2))], -1))
    sink_b = f(np.broadcast_to(inputs["sink"][:, None, :], (DEPTH, 128, 6)))
    C, S_ = _rope_tables(cfg, 0)
    MA = _mask_A(cfg, 0)
    MB = np.stack([_mask_B(cfg, 0, inputs["rpb"][l]) for l in range(DEPTH)])
    shared = {
        "w_ada": f(inputs["w_ada"]), "b_ada2": b_ada2, "w_in_p": w_in_p, "gains": gains,
        "ropeC": C, "ropeS": S_, "ident_f": ident, "pm_f": pm, "bd_f": bd,
        "MA_d": MA, "MB_d": MB, "sink_b": sink_b, "w_out_p": w_out_p, "lnv": lnv,
        "w_router": f(inputs["w_router"]), "rbias": f(inputs["router_bias"][None, :]), "sel": sel,
        "w1": f(inputs["w1"]), "w3": f(inputs["w3"]), "w2": f(inputs["w2"]),
    }
    maps = []
    for b in range(inputs["x"].shape[0]):
        m = dict(shared)
        m["x_in"] = f(np.concatenate([inputs["x"][b], inputs["ctx"][b]], 0))
        m["cond"] = f(np.stack([inputs["c"][b].reshape(8, 128).T, inputs["c_ctx"].reshape(8, 128).T], -1))
        maps.append(m)
    return maps


def build_qkv(cfg):
    nc = bass.Bass("TRN2", target_bir_lowering=False)
    with contextlib.ExitStack() as st:
        K = _new_ctx(nc, cfg, st)
        I, O = "ExternalInput", "ExternalOutput"
        x_d = _dt(K, "x_in", [cfg.NT, D], F32, I)
        cond = _dt(K, "cond", [128, 8, 2], F32, I)
        w_ada = _dt(K, "w_ada", [D, 6 * D], F32, I)
        b_ada2 = _dt(K, "b_ada2", [2, 6 * D], F32, I)
        w_in_p = _dt(K, "w_in_p", [D, 2048], F32, I)
        gains = _dt(K, "gains", [128, 2], F32, I)
        ropeC = _dt(K, "ropeC", [128, cfg.T], F32, I)
        ropeS = _dt(K, "ropeS", [128, cfg.T], F32, I)
        _dt(K, "ident_f", [128, 128], F32, I)
        pm_f = _dt(K, "pm_f", [128, 128], F32, I)
        bd_f = _dt(K, "bd_f", [128, 128], F32, I)
        mod_d = _dt(K, "mod_d", [2, 6 * D], F32, O)
        qT_d = _dt(K, "qT_d", [8, 128, cfg.NT], BF16, O)
        kT_d = _dt(K, "kT_d", [4, 128, cfg.NT], BF16, O)
        V_d = _dt(K, "V_d", [cfg.NT, 520], BF16, O)
        load_consts(K)
        phase_mod(K, w_ada, b_ada2, cond, mod_d)
        phase_qkv(K, x_d, mod_d, w_in_p, gains, ropeC, ropeS, pm_f, bd_f, qT_d,
                  lambda idx, tok0, nt, is_ctx: kT_d[idx][:, tok0:tok0 + nt],
                  lambda tok, is_ctx: [(V_d[tok:tok + 128, :], 0, 8)])
        K.P.emit(st)
        K.stats = (K.A.peak, K.P.n_sems, {e: len(K.P.ops[e]) for e in ENGS})
    return nc, K.stats


def build_rest(cfg):
    nc = bass.Bass("TRN2", target_bir_lowering=False)
    with contextlib.ExitStack() as st:
        K = _new_ctx(nc, cfg, st)
        I, O, N = "ExternalInput", "ExternalOutput", "Internal"
        x_d = _dt(K, "x_in", [cfg.NT, D], F32, I)
        mod_d = _dt(K, "mod_d", [2, 6 * D], F32, I)
        qT_d = _dt(K, "qT_d", [8, 128, cfg.NT], BF16, I)
        kTC_d = _dt(K, "kTC_d", [128, cfg.SA], BF16, I)
        VC_d = _dt(K, "VC_d", [cfg.SA, 130], BF16, I)
        kTAB_d = _dt(K, "kTAB_d", [3, 128, cfg.EXT], BF16, I)
        VAB_d = _dt(K, "VAB_d", [cfg.EXT, 390], BF16, I)
        kTABc_d = _dt(K, "kTABc_d", [3, 128, L], BF16, I)
        VABc_d = _dt(K, "VABc_d", [L, 390], BF16, I)
        MA_d = _dt(K, "MA_d", [cfg.NQT, 128, 3, 512], F32, I)
        MB_d = _dt(K, "MB_d", [4, 5, 128, 7, 128], F32, I)
        sink_b = _dt(K, "sink_b", [128, 6], F32, I)
        w_out_p = _dt(K, "w_out_p", [16, 64, D], F32, I)
        lnv = _dt(K, "lnv", [4, D], F32, I)
        w_router = _dt(K, "w_router", [D, NEXP], F32, I)
        rbias = _dt(K, "rbias", [1, NEXP], F32, I)
        sel_d = _dt(K, "sel", [16, 16 * 128], F32, I)
        w1 = _dt(K, "w1", [NEXP, D, DEXP], F32, I)
        w3 = _dt(K, "w3", [NEXP, D, DEXP], F32, I)
        w2 = _dt(K, "w2", [NEXP, DEXP, D], F32, I)
        _dt(K, "ident_f", [128, 128], F32, I)
        oT_d = _dt(K, "oT_d", [16, 64, cfg.NT], BF16, N)
        x1_d = _dt(K, "x1_d", [cfg.NT, D], F32, N)
        u2T_d = _dt(K, "u2T_d", [8, 128, cfg.NT], BF16, N)
        gT_d = _dt(K, "gT_d", [16, cfg.NT], F32, N)
        xo_d = _dt(K, "x_out", [cfg.NT, D], F32, O)
        load_consts(K)
        phase_attn(K, qT_d, kTC_d, VC_d, kTAB_d, VAB_d, kTABc_d, VABc_d, MA_d, MB_d, sink_b, oT_d)
        phase_proj(K, x_d, mod_d, oT_d, w_out_p, lnv, w_router, rbias, x1_d, u2T_d, gT_d)
        phase_moe(K, x1_d, mod_d, u2T_d, gT_d, [w1[e] for e in range(NEXP)], [w3[e] for e in range(NEXP)],
                  [w2[e] for e in range(NEXP)], lnv, sel_d, xo_d)
        K.P.emit(st)
        K.stats = (K.A.peak, K.P.n_sems, {e: len(K.P.ops[e]) for e in ENGS})
    return nc, K.stats


def host_qkv_inputs(cfg, inputs, l, x_cur, h_ctx):
    ident, pm, bd, _ = _consts()
    perm = _perm_w_in()
    w_in_p = np.ascontiguousarray(inputs["w_in"][l][:, perm])
    b_ada2 = np.ascontiguousarray(np.stack([inputs["b_ada"][l]] * 2))
    gains = np.ascontiguousarray(np.stack([np.tile(inputs["q_gain"][l], 2), np.tile(inputs["k_gain"][l], 2)], -1), np.float32)
    maps = []
    for core in range(8):
        b, q = core // 4, core % 4
        x_in = np.concatenate([x_cur[b, q * cfg.T:(q + 1) * cfg.T], h_ctx[b]], 0)
        cond = np.stack([inputs["c"][b].reshape(8, 128).T, inputs["c_ctx"].reshape(8, 128).T], -1)
        C, S_ = _rope_tables(cfg, q)
        maps.append({
            "x_in": np.ascontiguousarray(x_in, np.float32),
            "cond": np.ascontiguousarray(cond, np.float32),
            "w_ada": inputs["w_ada"][l], "b_ada2": b_ada2, "w_in_p": w_in_p, "gains": gains,
            "ropeC": C, "ropeS": S_, "ident_f": ident, "pm_f": pm, "bd_f": bd,
        })
    return maps


def host_rest_inputs(cfg, inputs, l, x_cur, h_ctx, qres):
    ident, pm, bd, sel = _consts()
    T = cfg.T
    w_out_p = np.ascontiguousarray(inputs["w_out"][l][_perm_w_out()])
    lnv = np.ascontiguousarray(np.stack([inputs["ln1_g"][l], inputs["ln1_b"][l], inputs["ln2_g"][l], inputs["ln2_b"][l]]))
    sink_b = np.ascontiguousarray(np.broadcast_to(inputs["sink"][l][None, :], (128, 6)), np.float32)
    rbias = np.ascontiguousarray(inputs["router_bias"][None, :])
    maps = []
    for core in range(8):
        b, q = core // 4, core % 4
        cores_b = [b * 4 + i for i in range(4)]
        kT = [np.asarray(qres[c]["kT_d"]) for c in cores_b]
        V = [np.asarray(qres[c]["V_d"]) for c in cores_b]
        kTC = np.concatenate([k[3][:, :T] for k in kT] + [kT[q][3][:, T:]], 1)
        VC = np.concatenate([v[:T, 390:520] for v in V] + [V[q][T:, 390:520]], 0)
        kfull = np.concatenate([k[0:3][:, :, :T] for k in kT], 2)
        kfull = np.pad(kfull, ((0, 0), (0, 0), (HALO, HALO)))
        kTAB = kfull[:, :, q * T:q * T + cfg.EXT]
        vfull = np.concatenate([v[:T, 0:390] for v in V], 0)
        vfull = np.pad(vfull, ((HALO, HALO), (0, 0)))
        VAB = vfull[q * T:q * T + cfg.EXT]
        x_in = np.concatenate([x_cur[b, q * T:(q + 1) * T], h_ctx[b]], 0)
        maps.append({
            "x_in": np.ascontiguousarray(x_in, np.float32),
            "mod_d": np.asarray(qres[core]["mod_d"]),
            "qT_d": np.asarray(qres[core]["qT_d"]),
            "kTC_d": np.ascontiguousarray(kTC), "VC_d": np.ascontiguousarray(VC),
            "kTAB_d": np.ascontiguousarray(kTAB), "VAB_d": np.ascontiguousarray(VAB),
            "kTABc_d": np.ascontiguousarray(kT[q][0:3][:, :, T:]), "VABc_d": np.ascontiguousarray(V[q][T:, 0:390]),
            "MA_d": _mask_A(cfg, q), "MB_d": _mask_B(cfg, q, inputs["rpb"][l]),
            "sink_b": sink_b, "w_out_p": w_out_p, "lnv": lnv,
            "w_router": inputs["w_router"], "rbias": rbias, "sel": sel,
            "w1": inputs["w1"][l], "w3": inputs["w3"][l], "w2": inputs["w2"][l],
            "ident_f": ident,
        })
    return maps


_PROGS = {}


def _get_prog(kind, cfg):
    key = (kind, cfg.S, cfg.NQ)
    if key not in _PROGS:
        _PROGS[key] = (build_qkv(cfg) if kind == "qkv" else build_rest(cfg))[0]
    return _PROGS[key]


def kernel(**inputs):
    inputs = {k: np.asarray(v) for k, v in inputs.items()}
    S = inputs["x"].shape[1]
    cfg = Cfg(S, NQ=4)
    x_cur = np.array(inputs["x"], dtype=np.float32)
    h_ctx = np.array(inputs["ctx"], dtype=np.float32)
    cores = list(range(8))
    for l in range(DEPTH):
        qres = run_bass_kernel_spmd(_get_prog("qkv", cfg), host_qkv_inputs(cfg, inputs, l, x_cur, h_ctx), core_ids=cores).results
        rres = run_bass_kernel_spmd(_get_prog("rest", cfg), host_rest_inputs(cfg, inputs, l, x_cur, h_ctx, qres), core_ids=cores).results
        x_new = np.empty_like(x_cur)
        h_new = np.empty_like(h_ctx)
        for core in cores:
            b, q = core // 4, core % 4
            xo = np.asarray(rres[core]["x_out"])
            x_new[b, q * cfg.T:(q + 1) * cfg.T] = xo[:cfg.T]
            if q == 0:
                h_new[b] = xo[cfg.T:]
        x_cur, h_ctx = x_new, h_new
    return x_cur
```

```python
import contextlib
import numpy as np
import ml_dtypes
import concourse.bass as bass
import concourse.mybir as mybir
from concourse.bass_utils import run_bass_kernel_spmd

F32 = mybir.dt.float32
BF16 = mybir.dt.bfloat16
ALU = mybir.AluOpType
AF = mybir.ActivationFunctionType
AX = mybir.AxisListType
NPBF = ml_dtypes.bfloat16

ENGS = ("pe", "act", "dve", "pool", "sp")
SEM_ROT = 1 << 20


class Res:
    __slots__ = ("name", "w", "rs_c", "rs_d")

    def __init__(self, name=""):
        self.name = name
        self.w = None
        self.rs_c = {}
        self.rs_d = []


class Op:
    __slots__ = ("eng", "idx", "fn", "deps", "dma", "signal", "sem", "val", "inc")

    def __init__(self, eng, idx, fn):
        self.eng = eng
        self.idx = idx
        self.fn = fn
        self.deps = ()
        self.dma = None
        self.signal = False
        self.sem = None
        self.val = 0
        self.inc = 16


class Prog:
    def __init__(self, nc):
        self.nc = nc
        self.ops = {e: [] for e in ENGS}
        self.streams = {}

    def res(self, name=""):
        return Res(name)

    def add(self, eng, fn, reads=(), writes=(), dma=None, inc=16):
        op = Op(eng, len(self.ops[eng]), fn)
        op.inc = inc
        deps = {}
        for r in reads:
            if r.w is not None:
                deps[id(r.w)] = r.w
        for w in writes:
            if w.w is not None:
                deps[id(w.w)] = w.w
            for o in w.rs_c.values():
                deps[id(o)] = o
            for o in w.rs_d:
                deps[id(o)] = o
        if dma is not None:
            st = self.streams.setdefault(dma, {"n": 0, "last": None})
            if st["last"] is not None:
                deps[id(st["last"])] = st["last"]
            st["n"] += 1
            op.dma = (dma, st["n"])
            st["last"] = op
        op.deps = tuple(deps.values())
        for r in reads:
            if op.dma is not None:
                r.rs_d.append(op)
            else:
                r.rs_c[eng] = op
        for w in writes:
            w.w = op
            w.rs_c = {}
            w.rs_d = []
        self.ops[eng].append(op)
        return op

    def barrier(self):
        lasts = []
        for e in ENGS:
            comp = [o for o in self.ops[e] if o.dma is None and o.fn is not None]
            if comp:
                lasts.append(comp[-1])
        dmas = [st["last"] for st in self.streams.values() if st["last"] is not None]
        for e in ENGS:
            op = Op(e, len(self.ops[e]), None)
            op.deps = tuple(lasts + dmas)
            self.ops[e].append(op)

    def emit(self, stack):
        nc = self.nc
        for e in ENGS:
            for op in self.ops[e]:
                for d in op.deps:
                    if d.dma is not None:
                        continue
                    if d.eng == op.eng and d.eng == "pe":
                        continue
                    d.signal = True
        nsem = 0
        for e in ENGS:
            cnt = 0
            cur = None
            for op in self.ops[e]:
                if op.dma is not None or not op.signal or op.fn is None:
                    continue
                if cur is None or cnt >= SEM_ROT:
                    cur = stack.enter_context(nc.semaphore(f"s_{e}_{nsem}"))
                    nsem += 1
                    cnt = 0
                cnt += 1
                op.sem = cur
                op.val = cnt
        stream_sems = {}
        for k in self.streams:
            stream_sems[k] = stack.enter_context(nc.semaphore(f"d_{nsem}"))
            nsem += 1
        for e in ENGS:
            for op in self.ops[e]:
                if op.dma is not None:
                    op.sem = stream_sems[op.dma[0]]
                    op.val = op.inc * op.dma[1]
        self.n_sems = nsem

        def run(e, h):
            known = {}
            for op in self.ops[e]:
                waits = {}
                for d in op.deps:
                    if d.dma is None:
                        if d.eng == e and e == "pe":
                            continue
                        if d.sem is None:
                            continue
                    key = id(d.sem)
                    if key not in waits or waits[key][1] < d.val:
                        waits[key] = (d.sem, d.val)
                for key, (sem, val) in waits.items():
                    if known.get(key, 0) >= val:
                        continue
                    known[key] = val
                    h.wait_ge(sem, val)
                if op.fn is None:
                    continue
                ins = op.fn(h)
                if op.dma is not None:
                    ins.then_inc(op.sem, op.inc)
                elif op.signal:
                    ins.then_inc(op.sem, 1)

        with nc.Block() as block:
            @block.tensor
            def _(h):
                run("pe", h)

            @block.scalar
            def _(h):
                run("act", h)

            @block.vector
            def _(h):
                run("dve", h)

            @block.gpsimd
            def _(h):
                run("pool", h)

            @block.sync
            def _(h):
                run("sp", h)


class SbufAlloc:
    def __init__(self, pool_ap, nbytes):
        self.pool = pool_ap
        self.nbytes = nbytes
        self.off = 0
        self.marks = []
        self.peak = 0

    def push(self):
        self.marks.append(self.off)

    def pop(self):
        self.off = self.marks.pop()

    def alloc(self, shape, dtype, parts=128):
        esz = 4 if dtype == F32 else 2
        n = int(np.prod(shape))
        nb = (n * esz + 31) // 32 * 32
        assert self.off + nb <= self.nbytes, f"SBUF overflow {self.off}+{nb}>{self.nbytes}"
        a = self.pool[0:parts, self.off // 4:(self.off + nb) // 4]
        self.off += nb
        self.peak = max(self.peak, self.off)
        if dtype != F32:
            a = a.bitcast(dtype)
        a = a[:, 0:n]
        if len(shape) == 2:
            a = a.rearrange("p (a b) -> p a b", b=shape[1])
        elif len(shape) == 3:
            a = a.rearrange("p (a b c) -> p a b c", b=shape[1], c=shape[2])
        return a


D = 1024
L = 256
HD = 64
GRID_W = 64
NA_ROWS, NA_COLS = 8, 16
NEXP = 16
DEXP = 512
DEPTH = 4
ALPHA = (2 * DEPTH) ** 0.25
LN_EPS = 1e-6
QK_EPS = 1e-6
NEG = -30000.0
SCALE = HD ** -0.5
HALO = 384
SBUF_BYTES = 204 * 1024


class Cfg:
    def __init__(self, S, NQ=4):
        self.S = S
        self.NQ = NQ
        self.T = S // NQ
        self.NT = self.T + L
        self.NQT = self.T // 512
        self.NQB = self.T // 128
        self.EXT = self.T + 2 * HALO
        self.SA = S + L
        self.NKT = self.SA // 128
        self.NBLK = S // 128
        self.NST = self.NT // 128


class Ctx:
    pass


def _bank(K, i, n=512):
    return K.ps[:, i * 512:i * 512 + n]


def _dma(K, out, in_, reads, writes, key, eng="sp"):
    return K.P.add(eng, lambda h: h.dma_start(out=out, in_=in_), reads, writes, dma=key)


def _mm(K, out, lhsT, rhs, start, stop, reads, writes):
    return K.P.add("pe", lambda h: h.matmul(out, lhsT=lhsT, rhs=rhs, start=start, stop=stop), reads, writes)


def _act(K, out, in_, func, reads, writes, scale=1.0, bias=0.0):
    return K.P.add("act", lambda h: h.activation(out=out, in_=in_, func=func, bias=bias, scale=scale), reads, writes)


def _tt(K, out, in0, in1, op, reads, writes, eng="dve"):
    return K.P.add(eng, lambda h: h.tensor_tensor(out=out, in0=in0, in1=in1, op=op), reads, writes)


def _stt(K, out, in0, scalar, in1, op0, op1, reads, writes, eng="dve"):
    return K.P.add(eng, lambda h: h.scalar_tensor_tensor(out=out, in0=in0, scalar=scalar, in1=in1, op0=op0, op1=op1), reads, writes)


def _ts(K, out, in0, s1, s2, op0, op1, reads, writes, eng="dve"):
    if s2 is None:
        return K.P.add(eng, lambda h: h.tensor_scalar(out=out, in0=in0, scalar1=s1, scalar2=None, op0=op0), reads, writes)
    return K.P.add(eng, lambda h: h.tensor_scalar(out=out, in0=in0, scalar1=s1, scalar2=s2, op0=op0, op1=op1), reads, writes)


def _copy(K, out, in_, reads, writes, eng="dve"):
    return K.P.add(eng, lambda h: h.tensor_copy(out=out, in_=in_), reads, writes)


def _bcast_row(K, dram_row_ap, name):
    n = dram_row_ap.shape[-1]
    t = K.A.alloc((n,), F32)
    r = K.P.res(name)
    _dma(K, t, dram_row_ap.broadcast_to([128, n]), [K.r_dram[dram_row_ap.tensor.name]] if dram_row_ap.tensor.name in K.r_dram else [], [r], "bc")
    return t, r


def load_consts(K):
    A, P, d = K.A, K.P, K.d
    K.ident_f = A.alloc((128,), F32)
    K.r_ident_f = P.res()
    _dma(K, K.ident_f, d["ident_f"], [], [K.r_ident_f], "c0")
    K.ident_b = A.alloc((128,), BF16)
    K.r_ident_b = P.res()
    _copy(K, K.ident_b, K.ident_f, [K.r_ident_f], [K.r_ident_b])
    K.ones_f = A.alloc((64,), F32)
    K.r_ones = P.res()
    P.add("dve", lambda h: h.memset(K.ones_f, 1.0), [], [K.r_ones])


def phase_mod(K, w_ada, b_ada2, cond, mod_d):
    A, P = K.A, K.P
    A.push()
    r_mod = K.r_dram[mod_d.tensor.name]
    cond_sb = A.alloc((8, 2), F32)
    r_cond = P.res()
    _dma(K, cond_sb, cond, [], [r_cond], "m0")
    condT = A.alloc((8, 2), F32)
    r_condT = P.res()
    _act(K, condT, cond_sb, AF.Silu, [r_cond], [r_condT])
    bada = A.alloc((6144,), F32, parts=2)
    r_bada = P.res()
    _dma(K, bada, b_ada2, [], [r_bada], "m1")
    modrow = A.alloc((6144,), F32, parts=2)
    r_modrow = P.res()
    wst = [A.alloc((8, 512), F32) for _ in range(2)]
    r_wst = [P.res() for _ in range(2)]
    for cb in range(12):
        s = cb % 2
        _dma(K, wst[s], w_ada[:, cb * 512:(cb + 1) * 512].rearrange("(k p) n -> p k n", p=128), [], [r_wst[s]], f"mw{s}")
        bk = _bank(K, s)
        for k in range(8):
            _mm(K, bk[0:2, :], condT[:, k, :], wst[s][:, k, :], k == 0, k == 7, [r_condT, r_wst[s]], [K.r_ps[s]])
        _tt(K, modrow[:, cb * 512:(cb + 1) * 512], bk[0:2, :], bada[:, cb * 512:(cb + 1) * 512], ALU.add,
            [K.r_ps[s], r_bada], [r_modrow])
    for lo, hi in ((1024, 3072), (4096, 6144)):
        _ts(K, modrow[:, lo:hi], modrow[:, lo:hi], 1.0, None, ALU.add, None, [r_modrow], [r_modrow])
    _dma(K, mod_d, modrow, [r_modrow], [r_mod], "m2")
    P.barrier()
    A.pop()


def phase_qkv(K, x_d, mod_d, w_in_p, gains, ropeC, ropeS, pm_f_d, bd_f_d, qT_d, kdst, vdst):
    A, P, cfg = K.A, K.P, K.cfg
    A.push()
    r_x = K.r_dram[x_d.tensor.name]
    r_q = K.r_dram[qT_d.tensor.name]
    pm_f = A.alloc((128,), F32)
    bd_f = A.alloc((128,), F32)
    r_pm, r_bd = P.res(), P.res()
    _dma(K, pm_f, pm_f_d, [], [r_pm], "q0")
    _dma(K, bd_f, bd_f_d, [], [r_bd], "q1")
    gn = A.alloc((2,), F32)
    r_gn = P.res()
    _dma(K, gn, gains, [], [r_gn], "q2")
    sh_o, r_sh_o = _bcast_row(K, mod_d[0:1, 0:1024], "sh1o")
    sc_o, r_sc_o = _bcast_row(K, mod_d[0:1, 1024:2048], "sc1o")
    sh_c, r_sh_c = _bcast_row(K, mod_d[1:2, 0:1024], "sh1c")
    sc_c, r_sc_c = _bcast_row(K, mod_d[1:2, 1024:2048], "sc1c")
    wbf = A.alloc((8, 2048), BF16)
    r_wbf = P.res()
    wst = [A.alloc((8, 512), F32) for _ in range(2)]
    r_wst = [P.res() for _ in range(2)]
    for cb in range(4):
        s = cb % 2
        _dma(K, wst[s], w_in_p[:, cb * 512:(cb + 1) * 512].rearrange("(k p) n -> p k n", p=128), [], [r_wst[s]], f"qw{s}")
        _act(K, wbf[:, :, cb * 512:(cb + 1) * 512], wst[s], AF.Copy, [r_wst[s]], [r_wbf])
    xs = [A.alloc((1024,), F32) for _ in range(2)]
    r_xs = [P.res() for _ in range(2)]
    tmp = A.alloc((1024,), F32)
    r_tmp = P.res()
    ub = [A.alloc((1024,), BF16) for _ in range(2)]
    r_ub = [P.res() for _ in range(2)]
    uT = [A.alloc((8, 512), BF16) for _ in range(2)]
    r_uT = [P.res() for _ in range(2)]
    rC = [A.alloc((512,), F32) for _ in range(2)]
    rS = [A.alloc((512,), F32) for _ in range(2)]
    r_rope = [P.res() for _ in range(2)]
    qf = [A.alloc((512,), F32) for _ in range(2)]
    r_qf = [P.res() for _ in range(2)]
    sq = A.alloc((512,), F32)
    r_sq = P.res()
    rt = A.alloc((512,), F32)
    r_rt = P.res()
    qn = A.alloc((512,), F32)
    r_qn = P.res()
    t1 = A.alloc((512,), F32)
    r_t1 = P.res()
    t2 = A.alloc((512,), F32)
    r_t2 = P.res()
    ob = [A.alloc((512,), BF16) for _ in range(3)]
    r_ob = [P.res() for _ in range(3)]
    vaug = [A.alloc((8, 65), BF16) for _ in range(2)]
    r_vaug = [P.res() for _ in range(2)]
    for s in range(2):
        P.add("pool", lambda h, s=s: h.memset(vaug[s], 1.0), [], [r_vaug[s]])

    tiles = [(t * 512, 512, False) for t in range(cfg.NQT)] + [(cfg.T, L, True)]
    sub_i = 0
    ob_i = 0
    for ti, (tok0, nt, is_ctx) in enumerate(tiles):
        tp = ti % 2
        sc, sh = (sc_c, sh_c) if is_ctx else (sc_o, sh_o)
        r_sc, r_sh = (r_sc_c, r_sh_c) if is_ctx else (r_sc_o, r_sh_o)
        nsub = nt // 128
        for st in range(nsub):
            s = sub_i % 2
            sub_i += 1
            _dma(K, xs[s], x_d[tok0 + st * 128: tok0 + (st + 1) * 128, :], [r_x], [r_xs[s]], f"qx{s}")
            _tt(K, tmp, xs[s], sc, ALU.mult, [r_xs[s], r_sc], [r_tmp])
            _tt(K, ub[s], tmp, sh, ALU.add, [r_tmp, r_sh], [r_ub[s]])
            bkT = _bank(K, s).bitcast(BF16)
            for k in range(8):
                P.add("pe", lambda h, k=k, s=s, bkT=bkT: h.transpose(out=bkT[:, k * 128:(k + 1) * 128], in_=ub[s][:, k * 128:(k + 1) * 128], identity=K.ident_b),
                      [r_ub[s], K.r_ident_b], [K.r_ps[s]])
            _act(K, uT[tp][:, :, st * 128:(st + 1) * 128], bkT.rearrange("p (a b) -> p a b", b=128), AF.Copy, [K.r_ps[s]], [r_uT[tp]])
        if not is_ctx:
            _dma(K, rC[tp][:, :nt], ropeC[:, tok0:tok0 + nt], [], [r_rope[tp]], f"qr{tp}")
            _dma(K, rS[tp][:, :nt], ropeS[:, tok0:tok0 + nt], [], [r_rope[tp]], f"qs{tp}")
        for ch in range(12):
            kind = ("rope", "rope", "rope", "plain", "plain", "norm", "norm", "norm", "rope", "plain", "plain", "norm")[ch]
            is_k = ch >= 8
            bq = 2 + (ch % 2)
            bkq = _bank(K, bq)
            for k in range(8):
                _mm(K, bkq[:, :nt], wbf[:, k, ch * 128:(ch + 1) * 128], uT[tp][:, k, :nt], k == 0, k == 7, [r_wbf, r_uT[tp]], [K.r_ps[bq]])
            o = ob_i % 3
            ob_i += 1
            if is_k:
                dst = kdst(ch - 8, tok0, nt, is_ctx)
                r_dst = K.r_dram[dst.tensor.name]
            else:
                dst = qT_d[ch][:, tok0:tok0 + nt]
                r_dst = r_q
            if kind == "plain" or (kind == "rope" and is_ctx):
                _act(K, ob[o][:, :nt], bkq[:, :nt], AF.Copy, [K.r_ps[bq]], [r_ob[o]])
            else:
                f = ch % 2
                _act(K, qf[f][:, :nt], bkq[:, :nt], AF.Copy, [K.r_ps[bq]], [r_qf[f]])
                src, r_src = qf[f], r_qf[f]
                bx = 4 + (ch % 2)
                bkx = _bank(K, bx)
                if kind == "norm":
                    _act(K, sq[:, :nt], bkq[:, :nt], AF.Square, [K.r_ps[bq]], [r_sq])
                    _mm(K, bkx[:, :nt], bd_f, sq[:, :nt], True, True, [r_bd, r_sq], [K.r_ps[bx]])
                    _act(K, rt[:, :nt], bkx[:, :nt], AF.Sqrt, [K.r_ps[bx]], [r_rt], scale=1.0 / HD, bias=QK_EPS)
                    P.add("dve", lambda h, nt=nt: h.reciprocal(out=rt[:, :nt], in_=rt[:, :nt]), [r_rt], [r_rt])
                    gcol = gn[:, 1:2] if is_k else gn[:, 0:1]
                    if is_ctx:
                        _stt(K, ob[o][:, :nt], qf[f][:, :nt], gcol, rt[:, :nt], ALU.mult, ALU.mult, [r_qf[f], r_gn, r_rt], [r_ob[o]])
                    else:
                        _stt(K, qn[:, :nt], qf[f][:, :nt], gcol, rt[:, :nt], ALU.mult, ALU.mult, [r_qf[f], r_gn, r_rt], [r_qn])
                        src, r_src = qn, r_qn
                if not is_ctx:
                    bx2 = 6 + (ch % 2)
                    bkx2 = _bank(K, bx2)
                    _mm(K, bkx2[:, :nt], pm_f, src[:, :nt], True, True, [r_pm, r_src], [K.r_ps[bx2]])
                    _tt(K, t1[:, :nt], src[:, :nt], rC[tp][:, :nt], ALU.mult, [r_src, r_rope[tp]], [r_t1])
                    _tt(K, t2[:, :nt], bkx2[:, :nt], rS[tp][:, :nt], ALU.mult, [K.r_ps[bx2], r_rope[tp]], [r_t2])
                    _tt(K, ob[o][:, :nt], t1[:, :nt], t2[:, :nt], ALU.add, [r_t1, r_t2], [r_ob[o]])
            _dma(K, dst, ob[o][:, :nt], [r_ob[o]], [r_dst], f"qo{o}")
        for st in range(nsub):
            s = st % 2
            bv = 4 + s
            bkv = _bank(K, bv)
            for k in range(8):
                _mm(K, bkv, uT[tp][:, k, st * 128:(st + 1) * 128], wbf[:, k, 1536:2048], k == 0, k == 7, [r_uT[tp], r_wbf], [K.r_ps[bv]])
            _act(K, vaug[s][:, :, 0:64], bkv.rearrange("p (a b) -> p a b", b=64), AF.Copy, [K.r_ps[bv]], [r_vaug[s]])
            for (vd, h0, h1) in vdst(tok0 + st * 128, is_ctx):
                _dma(K, vd, vaug[s][:, h0:h1, :].rearrange("p a b -> p (a b)"), [r_vaug[s]], [K.r_dram[vd.tensor.name]], f"qv{s}")
    P.barrier()
    A.pop()


def phase_attn(K, qT_d, kTC_d, VC_d, kTAB_d, VAB_d, kTABc_d, VABc_d, MA_d, MB_d, sink_b, oT_d):
    A, P, cfg = K.A, K.P, K.cfg
    A.push()
    rd = K.r_dram
    r_oT = rd[oT_d.tensor.name]
    rq = [rd[qT_d.tensor.name]]
    r_kvd = [rd[n.tensor.name] for n in (kTC_d, VC_d, kTAB_d, VAB_d, kTABc_d, VABc_d)]
    NKT, NQT, NQB, T = cfg.NKT, cfg.NQT, cfg.NQB, cfg.T

    snk = A.alloc((6,), F32)
    r_snk = P.res()
    _dma(K, snk, sink_b, [], [r_snk], "a0")
    esink = A.alloc((6,), F32)
    r_es = P.res()
    _act(K, esink, snk, AF.Exp, [r_snk], [r_es])

    kTC = A.alloc((cfg.SA,), BF16)
    r_kTC = P.res()
    nchunk = 4
    cw = cfg.SA // nchunk
    for i in range(nchunk):
        lo = i * cw
        hi = cfg.SA if i == nchunk - 1 else (i + 1) * cw
        _dma(K, kTC[:, lo:hi], kTC_d[:, lo:hi], r_kvd, [r_kTC], "a1")
    VC = A.alloc((NKT, 130), BF16)
    r_VC = P.res()
    vch = 13 if NKT % 13 == 0 else NKT
    for i in range(NKT // vch):
        _dma(K, VC[:, i * vch:(i + 1) * vch, :], VC_d[i * vch * 128:(i + 1) * vch * 128, :].rearrange("(t p) c -> p t c", p=128), r_kvd, [r_VC], "a2")
    kTABc = A.alloc((3, L), BF16)
    r_kTABc = P.res()
    _dma(K, kTABc, kTABc_d.rearrange("c p n -> p c n"), r_kvd, [r_kTABc], "a3")
    VABc = A.alloc((2, 390), BF16)
    r_VABc = P.res()
    _dma(K, VABc, VABc_d.rearrange("(t p) c -> p t c", p=128), r_kvd, [r_VABc], "a4")

    qT = [A.alloc((8, 512), BF16) for _ in range(2)]
    r_qT = [P.res() for _ in range(2)]
    kTABw = [A.alloc((3, 1280), BF16) for _ in range(2)]
    VABw = [A.alloc((10, 390), BF16) for _ in range(2)]
    MA = [A.alloc((3, 512), F32) for _ in range(2)]
    r_win = [P.res() for _ in range(2)]
    MBt = [A.alloc((7, 512), F32) for _ in range(2)]
    r_MBt = [P.res() for _ in range(2)]
    Pt = [A.alloc((1024,), BF16) for _ in range(3)]
    r_Pt = [P.res() for _ in range(3)]
    tmpm = [A.alloc((512,), F32) for _ in range(2)]
    r_tmpm = [P.res() for _ in range(2)]
    rs = [A.alloc((512,), F32) for _ in range(2)]
    r_rs = [P.res() for _ in range(2)]
    Bs = [A.alloc((512,), F32) for _ in range(2)]
    r_Bs = [P.res() for _ in range(2)]
    osb = [A.alloc((512,), BF16) for _ in range(2)]
    r_osb = [P.res() for _ in range(2)]
    cnt = {"pt": 0, "nm": 0, "mb": 0, "sb": 0, "ob": 0, "tm": 0}

    pending_norm = []

    def flush_norm():
        while pending_norm:
            pending_norm.pop(0)()

    def normalize(obank, nq, slot, tok0, es_col):
        pending_norm.append(lambda: normalize_now(obank, nq, slot, tok0, es_col))

    def normalize_now(obank, nq, slot, tok0, es_col):
        i = cnt["nm"] % 2
        cnt["nm"] += 1
        ob_ap = _bank(K, obank)
        if es_col is not None:
            _ts(K, rs[i][64:65, :nq], ob_ap[64:65, :nq], esink[64:65, es_col:es_col + 1], None, ALU.add, None, [K.r_ps[obank], r_es], [r_rs[i]])
            P.add("dve", lambda h: h.reciprocal(out=rs[i][64:65, :nq], in_=rs[i][64:65, :nq]), [r_rs[i]], [r_rs[i]])
        else:
            P.add("dve", lambda h: h.reciprocal(out=rs[i][64:65, :nq], in_=ob_ap[64:65, :nq]), [K.r_ps[obank]], [r_rs[i]])
        bb = 6 + i
        bp = _bank(K, bb)
        _mm(K, bp[0:64, :nq], K.ones_f[64:65, 0:64], rs[i][64:65, :nq], True, True, [K.r_ones, r_rs[i]], [K.r_ps[bb]])
        _act(K, Bs[i][0:64, :nq], bp[0:64, :nq], AF.Copy, [K.r_ps[bb]], [r_Bs[i]])
        _tt(K, osb[i][0:64, :nq], ob_ap[0:64, :nq], Bs[i][0:64, :nq], ALU.mult, [K.r_ps[obank], r_Bs[i]], [r_osb[i]])
        _dma(K, oT_d[slot, :, tok0:tok0 + nq], osb[i][0:64, :nq], [r_osb[i]], [r_oT], f"ao{i}")

    def cls_of(lb):
        if lb == 0:
            return 0
        if lb == 1:
            return 1
        if lb == NQB - 2:
            return 3
        if lb == NQB - 1:
            return 4
        return 2

    def attn_ab(tp, t, nq, is_ctx, tok0, c, half, group):
        pb = 64 * half
        if group == "A":
            kc, hv, offs, slot, es_col, moff = 0, half, (-1, 0, 1), 2 * c + half, c + 3 * half, 1
        else:
            hB = 2 * (c - 3) + half
            offs_b = (-3, -2, -1, 0, 1, 2, 3) if (t is None or t == 0 or t == NQT - 1) else (-2, -1, 0, 1, 2)
            kc, hv, offs, slot, es_col, moff = 1 + (c - 3), 2 + hB, offs_b, 2 * c + half, None, 3
        obank = cnt["ob"] % 2
        cnt["ob"] += 1
        ob_ap = _bank(K, obank)
        r_ob = K.r_ps[obank]
        nsteps = 2 + (0 if is_ctx else len(offs))
        step = 0
        if group == "B" and not is_ctx:
            mi = cnt["mb"] % 2
            cnt["mb"] += 1
            for b in range(4):
                _dma(K, MBt[mi][:, :, b * 128:(b + 1) * 128], MB_d[hB, cls_of(4 * t + b)], [], [r_MBt[mi]], f"am{mi}")
        steps = [("ctx", cb) for cb in range(2)]
        if not is_ctx:
            steps += [("win", oi) for oi in range(len(offs))]
        pend = []

        def pv(st_i):
            kind, a, pi = pend[st_i]
            last = st_i == len(steps) - 1
            if kind == "ctx":
                _mm(K, ob_ap[0:65, :nq], VABc[:, a, hv * 65:(hv + 1) * 65], Pt[pi][:, :nq], st_i == 0, last, [r_VABc, r_Pt[pi]], [r_ob])
            else:
                off = offs[a]
                for b in range(4):
                    wb = b + 3 + off
                    _mm(K, ob_ap[0:65, b * 128:(b + 1) * 128], VABw[tp][:, wb, hv * 65:(hv + 1) * 65], Pt[pi][:, b * 128:(b + 1) * 128],
                        False, (last and b == 3), [r_win[tp], r_Pt[pi]], [r_ob])

        for st_i, (kind, a) in enumerate(steps):
            sbk = 2 + (cnt["sb"] % 2) * 2
            cnt["sb"] += 1
            sb_ap = _bank(K, sbk)
            pi = cnt["pt"] % 3
            cnt["pt"] += 1
            if kind == "ctx":
                _mm(K, sb_ap[:, :nq], kTABc[pb:pb + 64, kc, a * 128:(a + 1) * 128], qT[tp][pb:pb + 64, c, :nq], True, True,
                    [r_kTABc, r_qT[tp]], [K.r_ps[sbk]])
                _act(K, Pt[pi][:, :nq], sb_ap[:, :nq], AF.Exp, [K.r_ps[sbk]], [r_Pt[pi]], scale=SCALE)
            else:
                off = offs[a]
                for b in range(4):
                    wb = b + 3 + off
                    _mm(K, sb_ap[:, b * 128:(b + 1) * 128], kTABw[tp][pb:pb + 64, kc, wb * 128:(wb + 1) * 128],
                        qT[tp][pb:pb + 64, c, b * 128:(b + 1) * 128], True, True, [r_win[tp], r_qT[tp]], [K.r_ps[sbk]])
                mi2 = cnt["tm"] % 2
                cnt["tm"] += 1
                if group == "A":
                    m_ap, r_m = MA[tp][:, off + moff, :], r_win[tp]
                else:
                    m_ap, r_m = MBt[mi][:, off + moff, :], r_MBt[mi]
                _stt(K, tmpm[mi2], sb_ap, SCALE, m_ap, ALU.mult, ALU.add, [K.r_ps[sbk], r_m], [r_tmpm[mi2]])
                _act(K, Pt[pi][:, :512], tmpm[mi2], AF.Exp, [r_tmpm[mi2]], [r_Pt[pi]])
            pend.append((kind, a, pi))
            if st_i == 0:
                flush_norm()
            if st_i > 0:
                pv(st_i - 1)
        pv(len(steps) - 1)
        normalize(obank, nq, slot, tok0, es_col)

    def attn_c(tp, nq, is_ctx, tok0, c):
        kts = list(range(NKT - 2, NKT)) if is_ctx else list(range(NKT))
        pis = []

        def pv(i):
            kt = kts[i]
            for half in range(2):
                _mm(K, _bank(K, half)[0:65, :nq], VC[:, kt, half * 65:(half + 1) * 65], Pt[pis[i]][:, half * 512:half * 512 + nq],
                    i == 0, i == len(kts) - 1, [r_VC, r_Pt[pis[i]]], [K.r_ps[half]])

        for i, kt in enumerate(kts):
            sp_ = cnt["sb"] % 2
            cnt["sb"] += 1
            sbk = 2 + sp_ * 2
            for half in range(2):
                pb = 64 * half
                _mm(K, _bank(K, sbk + half)[:, :nq], kTC[pb:pb + 64, kt * 128:(kt + 1) * 128], qT[tp][pb:pb + 64, c, :nq], True, True,
                    [r_kTC, r_qT[tp]], [K.r_ps[sbk + half]])
            pi = cnt["pt"] % 3
            cnt["pt"] += 1
            pis.append(pi)
            s2 = K.ps[:, sbk * 512:(sbk + 2) * 512].rearrange("p (a b) -> p a b", b=512)[:, :, :nq]
            p2 = Pt[pi].rearrange("p (a b) -> p a b", b=512)[:, :, :nq]
            _act(K, p2, s2, AF.Exp, [K.r_ps[sbk], K.r_ps[sbk + 1]], [r_Pt[pi]], scale=SCALE)
            if i == 0:
                flush_norm()
            if i > 0:
                pv(i - 1)
        pv(len(kts) - 1)
        for half in range(2):
            normalize(half, nq, 2 * c + half, tok0, None)

    tiles = [(t, t * 512, 512, False) for t in range(NQT)] + [(None, T, L, True)]
    for ti, (t, tok0, nq, is_ctx) in enumerate(tiles):
        tp = ti % 2
        _dma(K, qT[tp][:, :, :nq], qT_d[:, :, tok0:tok0 + nq].rearrange("c p n -> p c n"), rq, [r_qT[tp]], f"aq{tp}")
        if not is_ctx:
            _dma(K, kTABw[tp], kTAB_d[:, :, t * 512:t * 512 + 1280].rearrange("c p n -> p c n"), r_kvd, [r_win[tp]], f"aw{tp}")
            _dma(K, VABw[tp], VAB_d[t * 512:t * 512 + 1280, :].rearrange("(t p) c -> p t c", p=128), r_kvd, [r_win[tp]], f"av{tp}")
            _dma(K, MA[tp], MA_d[t], [], [r_win[tp]], f"ax{tp}")
        for c in range(3):
            for half in range(2):
                attn_ab(tp, t, nq, is_ctx, tok0, c, half, "A")
        for c in range(3, 5):
            for half in range(2):
                attn_ab(tp, t, nq, is_ctx, tok0, c, half, "B")
        for c in range(5, 8):
            attn_c(tp, nq, is_ctx, tok0, c)
        flush_norm()
    P.barrier()
    A.pop()


def _layer_norm(K, z, r_z, out, r_out, lng, r_lng, lnb, r_lnb, scr):
    P = K.P
    stt, mv, r_s = scr
    for i in range(2):
        P.add("dve", lambda h, i=i: h.bn_stats(out=stt[:, i, :], in_=z[:, i * 512:(i + 1) * 512]), [r_z], [r_s])
    P.add("dve", lambda h: h.bn_aggr(out=mv[:, 0:2], in_=stt.rearrange("p a b -> p (a b)")), [r_s], [r_s])
    _act(K, mv[:, 2:3], mv[:, 1:2], AF.Sqrt, [r_s], [r_s], scale=1.0, bias=LN_EPS)
    P.add("dve", lambda h: h.reciprocal(out=mv[:, 3:4], in_=mv[:, 2:3]), [r_s], [r_s])
    _ts(K, z, z, mv[:, 0:1], mv[:, 3:4], ALU.subtract, ALU.mult, [r_z, r_s], [r_z])
    _tt(K, z, z, lng, ALU.mult, [r_z, r_lng], [r_z])
    _tt(K, out, z, lnb, ALU.add, [r_z, r_lnb], [r_out])


def phase_proj(K, x_d, mod_d, oT_d, w_out_p, lnv, w_router, rbias, x1_d, u2T_d, gT_d):
    A, P, cfg = K.A, K.P, K.cfg
    A.push()
    rd = K.r_dram
    r_x, r_oT, r_x1, r_u2T, r_gT = rd[x_d.tensor.name], rd[oT_d.tensor.name], rd[x1_d.tensor.name], rd[u2T_d.tensor.name], rd[gT_d.tensor.name]
    T = cfg.T
    wo = A.alloc((16, 1024), BF16, parts=64)
    r_wo = P.res()
    wst = [A.alloc((4, 1024), F32, parts=64) for _ in range(2)]
    r_wst = [P.res() for _ in range(2)]
    for g in range(4):
        s = g % 2
        _dma(K, wst[s], w_out_p[g * 4:(g + 1) * 4].rearrange("s p n -> p s n"), [], [r_wst[s]], f"pw{s}")
        _act(K, wo[:, g * 4:(g + 1) * 4, :], wst[s], AF.Copy, [r_wst[s]], [r_wo])
    g1 = [_bcast_row(K, mod_d[i:i + 1, 2048:3072], "g1") for i in range(2)]
    sh2 = [_bcast_row(K, mod_d[i:i + 1, 3072:4096], "sh2") for i in range(2)]
    sc2 = [_bcast_row(K, mod_d[i:i + 1, 4096:5120], "sc2") for i in range(2)]
    lng, r_lng = _bcast_row(K, lnv[0:1, :], "lng1")
    lnb, r_lnb = _bcast_row(K, lnv[1:2, :], "lnb1")
    wr = A.alloc((8, 16), F32)
    r_wr = P.res()
    _dma(K, wr, w_router.rearrange("(k p) e -> p k e", p=128), [], [r_wr], "p0")
    rb, r_rb = _bcast_row(K, rbias, "rb")
    oTt = [A.alloc((16, 128), BF16, parts=64) for _ in range(2)]
    r_oTt = [P.res() for _ in range(2)]
    xs = [A.alloc((1024,), F32) for _ in range(2)]
    r_xs = [P.res() for _ in range(2)]
    tb2 = [A.alloc((1024,), F32) for _ in range(2)]
    r_tb2 = [P.res() for _ in range(2)]
    z2 = [A.alloc((1024,), F32) for _ in range(2)]
    r_z2 = [P.res() for _ in range(2)]
    x1 = [A.alloc((1024,), F32) for _ in range(2)]
    r_x1s = [P.res() for _ in range(2)]
    u22 = [A.alloc((1024,), F32) for _ in range(2)]
    r_u22 = [P.res() for _ in range(2)]
    u2b = [A.alloc((8, 128), BF16) for _ in range(2)]
    r_u2b = [P.res() for _ in range(2)]
    u2f2 = [A.alloc((8, 128), F32) for _ in range(2)]
    r_u2f2 = [P.res() for _ in range(2)]
    stt2 = [A.alloc((2, 6), F32) for _ in range(2)]
    mv2 = [A.alloc((8,), F32) for _ in range(2)]
    r_s2 = [P.res() for _ in range(2)]
    gsc = [[A.alloc((16,), F32) for _ in range(8)] for _ in range(2)]
    r_g2 = [P.res() for _ in range(2)]
    gate = [A.alloc((16,), F32) for _ in range(2)]
    r_gate = [P.res() for _ in range(2)]
    gTs = [A.alloc((128,), F32, parts=16) for _ in range(2)]
    r_gTs = [P.res() for _ in range(2)]

    def v44(a):
        return a.rearrange("p (a b) -> p a b", b=4)

    def _pre(st):
        s = st % 2
        tok = st * 128
        ic = 1 if tok >= T else 0
        return s, tok, ic

    def stage_a(st):
        s, tok, ic = _pre(st)
        tb, r_tb, z, r_z, u2, r_u2, u2f, r_u2f = tb2[s], r_tb2[s], z2[s], r_z2[s], u22[s], r_u22[s], u2f2[s], r_u2f2[s]
        stt, mv, r_s, r_g = stt2[s], mv2[s], r_s2[s], r_g2[s]
        g_aff, g_sel, g_eq, g_s2, g_m, g_ch = gsc[s][0], gsc[s][1], gsc[s][2], gsc[s][3], gsc[s][4], gsc[s][5]
        g_gm, g_dn = gsc[s][6][:, 0:4], gsc[s][7][:, 0:2]
        _dma(K, oTt[s], oT_d[:, :, tok:tok + 128].rearrange("s p n -> p s n"), [r_oT], [r_oTt[s]], f"po{s}")
        _dma(K, xs[s], x_d[tok:tok + 128, :], [r_x], [r_xs[s]], f"px{s}")
        for dh in range(2):
            for sl in range(16):
                _mm(K, _bank(K, dh), oTt[s][:, sl, :], wo[:, sl, dh * 512:(dh + 1) * 512], sl == 0, sl == 15, [r_oTt[s], r_wo], [K.r_ps[dh]])
        _tt(K, tb, K.ps[:, 0:1024], g1[ic][0], ALU.mult, [K.r_ps[0], K.r_ps[1], g1[ic][1]], [r_tb])
        _stt(K, z, xs[s], ALPHA, tb, ALU.mult, ALU.add, [r_xs[s], r_tb], [r_z])
        _layer_norm(K, z, r_z, x1[s], r_x1s[s], lng, r_lng, lnb, r_lnb, (stt, mv, r_s))
        _dma(K, x1_d[tok:tok + 128, :], x1[s], [r_x1s[s]], [r_x1], f"p1{s}")

    def stage_b(st):
        s, tok, ic = _pre(st)
        tb, r_tb, z, r_z, u2, r_u2, u2f, r_u2f = tb2[s], r_tb2[s], z2[s], r_z2[s], u22[s], r_u22[s], u2f2[s], r_u2f2[s]
        stt, mv, r_s, r_g = stt2[s], mv2[s], r_s2[s], r_g2[s]
        g_aff, g_sel, g_eq, g_s2, g_m, g_ch = gsc[s][0], gsc[s][1], gsc[s][2], gsc[s][3], gsc[s][4], gsc[s][5]
        g_gm, g_dn = gsc[s][6][:, 0:4], gsc[s][7][:, 0:2]
        _tt(K, u2, x1[s], sc2[ic][0], ALU.mult, [r_x1s[s], sc2[ic][1]], [r_u2])
        _tt(K, u2, u2, sh2[ic][0], ALU.add, [r_u2, sh2[ic][1]], [r_u2])
        for k in range(8):
            bk = 2 + k // 4
            _mm(K, _bank(K, bk)[:, (k % 4) * 128:(k % 4 + 1) * 128], u2[:, k * 128:(k + 1) * 128], K.ident_f, True, True,
                [r_u2, K.r_ident_f], [K.r_ps[bk]])
        tview = K.ps[:, 1024:2048].rearrange("p (a b) -> p a b", b=128)
        _act(K, u2f, tview, AF.Copy, [K.r_ps[2], K.r_ps[3]], [r_u2f])
        _act(K, u2b[s], u2f, AF.Copy, [r_u2f], [r_u2b[s]])
        _dma(K, u2T_d[:, :, tok:tok + 128].rearrange("k p n -> p k n"), u2b[s], [r_u2b[s]], [r_u2T], f"p2{s}")
        lg = _bank(K, 4)[:, 0:16]
        for k in range(8):
            _mm(K, lg, u2f[:, k, :], wr[:, k, :], k == 0, k == 7, [r_u2f, r_wr], [K.r_ps[4]])
        _act(K, g_aff, lg, AF.Sigmoid, [K.r_ps[4]], [r_g])
        _tt(K, g_sel, g_aff, rb, ALU.add, [r_g, r_rb], [r_g])
        P.add("dve", lambda h, g_m=g_m, g_sel=g_sel: h.tensor_reduce(out=g_m[:, 0:4], in_=v44(g_sel), axis=AX.X, op=ALU.max), [r_g], [r_g])
        _tt(K, v44(g_eq), v44(g_sel), g_m[:, 0:4].unsqueeze(2).to_broadcast([128, 4, 4]), ALU.is_equal, [r_g], [r_g])
        _stt(K, g_s2, g_eq, -1e9, g_sel, ALU.mult, ALU.add, [r_g], [r_g])
        P.add("dve", lambda h, g_m=g_m, g_s2=g_s2: h.tensor_reduce(out=g_m[:, 4:8], in_=v44(g_s2), axis=AX.X, op=ALU.max), [r_g], [r_g])
        _tt(K, g_m[:, 8:12], g_m[:, 0:4], g_m[:, 4:8], ALU.add, [r_g], [r_g])
        P.add("dve", lambda h, g_m=g_m: h.tensor_reduce(out=g_m[:, 12:13], in_=g_m[:, 8:12], axis=AX.X, op=ALU.max), [r_g], [r_g])
        _tt(K, g_gm, g_m[:, 8:12], g_m[:, 12:13].to_broadcast([128, 4]), ALU.is_equal, [r_g], [r_g])
        _tt(K, v44(g_ch), v44(g_sel), g_m[:, 4:8].unsqueeze(2).to_broadcast([128, 4, 4]), ALU.is_ge, [r_g], [r_g])
        _tt(K, v44(g_ch), v44(g_ch), g_gm.unsqueeze(2).to_broadcast([128, 4, 4]), ALU.mult, [r_g], [r_g])
        _tt(K, g_ch, g_ch, g_aff, ALU.mult, [r_g], [r_g])
        P.add("dve", lambda h, g_dn=g_dn, g_ch=g_ch: h.tensor_reduce(out=g_dn[:, 0:1], in_=g_ch, axis=AX.X, op=ALU.add), [r_g], [r_g])
        P.add("dve", lambda h, g_dn=g_dn: h.reciprocal(out=g_dn[:, 1:2], in_=g_dn[:, 0:1]), [r_g], [r_g])
        _ts(K, gate[s], g_ch, g_dn[:, 1:2], None, ALU.mult, None, [r_g], [r_gate[s]])
        _mm(K, _bank(K, 5)[0:16, 0:128], gate[s], K.ident_f, True, True, [r_gate[s], K.r_ident_f], [K.r_ps[5]])
        _act(K, gTs[s], _bank(K, 5)[0:16, 0:128], AF.Copy, [K.r_ps[5]], [r_gTs[s]])
        _dma(K, gT_d[:, tok:tok + 128], gTs[s], [r_gTs[s]], [r_gT], f"p3{s}")

    for st in range(cfg.NST + 1):
        if st < cfg.NST:
            stage_a(st)
        if st >= 1:
            stage_b(st - 1)
    P.barrier()
    A.pop()


def phase_moe(K, x1_d, mod_d, u2T_d, gT_d, w1, w3, w2, lnv, sel_d, xo_d):
    A, P, cfg = K.A, K.P, K.cfg
    A.push()
    rd = K.r_dram
    r_x1, r_u2T, r_gT, r_xo = rd[x1_d.tensor.name], rd[u2T_d.tensor.name], rd[gT_d.tensor.name], rd[xo_d.tensor.name]
    T, NST = cfg.T, cfg.NST
    g2 = [_bcast_row(K, mod_d[i:i + 1, 5120:6144], "g2") for i in range(2)]
    lng, r_lng = _bcast_row(K, lnv[2:3, :], "lng2")
    lnb, r_lnb = _bcast_row(K, lnv[3:4, :], "lnb2")
    sel = A.alloc((16, 128), F32, parts=16)
    r_sel = P.res()
    _dma(K, sel, sel_d.rearrange("k (e m) -> k e m", m=128), [], [r_sel], "e0")
    npass = (NST + 11) // 12
    nsm = (NST + npass - 1) // npass
    halves = [(i * nsm, min(NST, (i + 1) * nsm)) for i in range(npass)]
    uT2 = A.alloc((8, nsm * 128), BF16)
    r_uT2 = P.res()
    gTt = A.alloc((nsm * 128,), F32, parts=16)
    r_gTt = P.res()
    yacc = A.alloc((nsm, 1024), F32)
    r_yacc = P.res()
    w1b = [A.alloc((8, 512), BF16) for _ in range(2)]
    w3b = [A.alloc((8, 512), BF16) for _ in range(2)]
    w2b = [A.alloc((4, 1024), BF16) for _ in range(2)]
    r_wb = [P.res() for _ in range(2)]
    stg = [A.alloc((1024,), F32) for _ in range(2)]
    r_stg = [P.res() for _ in range(2)]
    hidb = [A.alloc((4, 512), BF16) for _ in range(2)]
    r_hidb = [P.res() for _ in range(2)]
    s_sb = [A.alloc((512,), F32) for _ in range(2)]
    r_ssb = [P.res() for _ in range(2)]
    t_sb = [A.alloc((512,), F32) for _ in range(2)]
    r_tsb = [P.res() for _ in range(2)]
    xs = [A.alloc((1024,), F32) for _ in range(2)]
    r_xs = [P.res() for _ in range(2)]
    tb = A.alloc((1024,), F32)
    r_tb = P.res()
    z = A.alloc((1024,), F32)
    r_z = P.res()
    xo = [A.alloc((1024,), F32) for _ in range(2)]
    r_xos = [P.res() for _ in range(2)]
    stt = A.alloc((2, 6), F32)
    mv = A.alloc((8,), F32)
    r_s = P.res()
    cnt = {"stg": 0, "h": 0, "y": 0, "g": 0, "grp": 0, "ex": 0}
    pending_y = []

    for (s_lo, s_hi) in halves:
        nsub = s_hi - s_lo
        if nsub <= 0:
            continue
        tok_lo = s_lo * 128
        ntok = nsub * 128
        _dma(K, uT2[:, :, :ntok], u2T_d[:, :, tok_lo:tok_lo + ntok].rearrange("k p n -> p k n"), [r_u2T], [r_uT2], "e1")
        _dma(K, gTt[:, :ntok], gT_d[:, tok_lo:tok_lo + ntok], [r_gT], [r_gTt], "e2")
        groups = [(g0, min(4, nsub - g0)) for g0 in range(0, nsub, 4)]
        for e in range(NEXP):
            wp = cnt["ex"] % 2
            cnt["ex"] += 1
            pieces = []
            for j in range(4):
                pieces.append((w1[e][j * 256:(j + 1) * 256, :].rearrange("(k p) n -> p k n", p=128), w1b[wp][:, 2 * j:2 * j + 2, :], 2, 512))
            for j in range(4):
                pieces.append((w3[e][j * 256:(j + 1) * 256, :].rearrange("(k p) n -> p k n", p=128), w3b[wp][:, 2 * j:2 * j + 2, :], 2, 512))
            for j in range(4):
                pieces.append((w2[e][j * 128:(j + 1) * 128, :].rearrange("(k p) n -> p k n", p=128), w2b[wp][:, j:j + 1, :], 1, 1024))
            for (src, dst, a, b) in pieces:
                si = cnt["stg"] % 2
                cnt["stg"] += 1
                sv = stg[si].rearrange("p (a b) -> p a b", b=b)
                _dma(K, sv, src, [], [r_stg[si]], f"es{si}")
                _copy(K, dst, sv, [r_stg[si]], [r_wb[wp]], eng="pool")
            for (g0, gn) in groups:
                n = gn * 128
                c0 = g0 * 128
                gbk = 4 + cnt["g"] % 2
                cnt["g"] += 1
                gb = _bank(K, gbk)
                _mm(K, gb[:, :n], sel[0:16, e, :], gTt[0:16, c0:c0 + n], True, True, [r_sel, r_gTt], [K.r_ps[gbk]])
                hp = cnt["grp"] % 2
                cnt["grp"] += 1
                for fc in range(4):
                    hb = cnt["h"] % 2
                    cnt["h"] += 1
                    b1, b3 = hb, 2 + hb
                    for k in range(8):
                        _mm(K, _bank(K, b1)[:, :n], w1b[wp][:, k, fc * 128:(fc + 1) * 128], uT2[:, k, c0:c0 + n], k == 0, k == 7, [r_wb[wp], r_uT2], [K.r_ps[b1]])
                    for k in range(8):
                        _mm(K, _bank(K, b3)[:, :n], w3b[wp][:, k, fc * 128:(fc + 1) * 128], uT2[:, k, c0:c0 + n], k == 0, k == 7, [r_wb[wp], r_uT2], [K.r_ps[b3]])
                    _act(K, s_sb[hb][:, :n], _bank(K, b1)[:, :n], AF.Silu, [K.r_ps[b1]], [r_ssb[hb]])
                    _tt(K, t_sb[hb][:, :n], s_sb[hb][:, :n], _bank(K, b3)[:, :n], ALU.mult, [r_ssb[hb], K.r_ps[b3]], [r_tsb[hb]])
                    _tt(K, hidb[hp][:, fc, :n], t_sb[hb][:, :n], gb[:, :n], ALU.mult, [r_tsb[hb], K.r_ps[gbk]], [r_hidb[hp]])
                def y_part(e=e, g0=g0, gn=gn, hp=hp, wp=wp):
                    for j in range(gn):
                        sub = g0 + j
                        for dh in range(2):
                            yb = 6 + cnt["y"] % 2
                            cnt["y"] += 1
                            for fc in range(4):
                                _mm(K, _bank(K, yb), hidb[hp][:, fc, j * 128:(j + 1) * 128], w2b[wp][:, fc, dh * 512:(dh + 1) * 512], fc == 0, fc == 3,
                                    [r_hidb[hp], r_wb[wp]], [K.r_ps[yb]])
                            ya = yacc[:, sub, dh * 512:(dh + 1) * 512]
                            if e == 0:
                                _copy(K, ya, _bank(K, yb), [K.r_ps[yb]], [r_yacc])
                            else:
                                _tt(K, ya, ya, _bank(K, yb), ALU.add, [r_yacc, K.r_ps[yb]], [r_yacc])
                while pending_y:
                    pending_y.pop(0)()
                pending_y.append(y_part)
        while pending_y:
            pending_y.pop(0)()
        for j in range(nsub):
            st = s_lo + j
            s = st % 2
            tok = st * 128
            ic = 1 if tok >= T else 0
            _dma(K, xs[s], x1_d[tok:tok + 128, :], [r_x1], [r_xs[s]], f"ex{s}")
            _tt(K, tb, yacc[:, j, :], g2[ic][0], ALU.mult, [r_yacc, g2[ic][1]], [r_tb])
            _stt(K, z, xs[s], ALPHA, tb, ALU.mult, ALU.add, [r_xs[s], r_tb], [r_z])
            _layer_norm(K, z, r_z, xo[s], r_xos[s], lng, r_lng, lnb, r_lnb, (stt, mv, r_s))
            _dma(K, xo_d[tok:tok + 128, :], xo[s], [r_xos[s]], [r_xo], f"ey{s}")
    P.barrier()
    A.pop()


def _new_ctx(nc, cfg, st):
    K = Ctx()
    K.nc = nc
    K.cfg = cfg
    pool = st.enter_context(nc.sbuf_tensor("pool", [128, SBUF_BYTES // 4], F32))
    ps = st.enter_context(nc.psum_tensor("ps", [128, 4096], F32))
    K.ps = ps[:]
    K.A = SbufAlloc(pool[:], SBUF_BYTES)
    K.P = Prog(nc)
    K.r_ps = [K.P.res(f"ps{i}") for i in range(8)]
    K.r_dram = {}
    K.d = {}
    return K


def _dt(K, name, shape, dtype, kind):
    t = K.nc.dram_tensor(name, list(shape), dtype, kind=kind).ap()
    K.d[name] = t
    K.r_dram[name] = K.P.res(name)
    return t


def build_fused(cfg, nlayers=DEPTH):
    assert cfg.NQ == 1
    nc = bass.Bass("TRN2", target_bir_lowering=False)
    T, NT, SA, EXT = cfg.T, cfg.NT, cfg.SA, cfg.EXT
    with contextlib.ExitStack() as st:
        K = _new_ctx(nc, cfg, st)
        A, P = K.A, K.P
        I, O, N = "ExternalInput", "ExternalOutput", "Internal"
        x_in = _dt(K, "x_in", [NT, D], F32, I)
        cond = _dt(K, "cond", [128, 8, 2], F32, I)
        w_ada = _dt(K, "w_ada", [DEPTH, D, 6 * D], F32, I)
        b_ada2 = _dt(K, "b_ada2", [DEPTH, 2, 6 * D], F32, I)
        w_in_p = _dt(K, "w_in_p", [DEPTH, D, 2048], F32, I)
        gains = _dt(K, "gains", [DEPTH, 128, 2], F32, I)
        ropeC = _dt(K, "ropeC", [128, T], F32, I)
        ropeS = _dt(K, "ropeS", [128, T], F32, I)
        _dt(K, "ident_f", [128, 128], F32, I)
        pm_f = _dt(K, "pm_f", [128, 128], F32, I)
        bd_f = _dt(K, "bd_f", [128, 128], F32, I)
        MA_d = _dt(K, "MA_d", [cfg.NQT, 128, 3, 512], F32, I)
        MB_d = _dt(K, "MB_d", [DEPTH, 4, 5, 128, 7, 128], F32, I)
        sink_b = _dt(K, "sink_b", [DEPTH, 128, 6], F32, I)
        w_out_p = _dt(K, "w_out_p", [DEPTH, 16, 64, D], F32, I)
        lnv = _dt(K, "lnv", [DEPTH, 4, D], F32, I)
        w_router = _dt(K, "w_router", [D, NEXP], F32, I)
        rbias = _dt(K, "rbias", [1, NEXP], F32, I)
        sel_d = _dt(K, "sel", [16, 16 * 128], F32, I)
        w1 = _dt(K, "w1", [DEPTH, NEXP, D, DEXP], F32, I)
        w3 = _dt(K, "w3", [DEPTH, NEXP, D, DEXP], F32, I)
        w2 = _dt(K, "w2", [DEPTH, NEXP, DEXP, D], F32, I)
        x_out = _dt(K, "x_out", [T, D], F32, O)
        x_d = _dt(K, "x_d", [NT, D], F32, N)
        mod_d = [_dt(K, f"mod_d{l}", [2, 6 * D], F32, N) for l in range(nlayers)]
        qT_d = _dt(K, "qT_d", [8, 128, NT], BF16, N)
        kTC_d = _dt(K, "kTC_d", [128, SA], BF16, N)
        VC_d = _dt(K, "VC_d", [SA, 130], BF16, N)
        kTAB_d = _dt(K, "kTAB_d", [3, 128, EXT], BF16, N)
        VAB_d = _dt(K, "VAB_d", [EXT, 390], BF16, N)
        kTABc_d = _dt(K, "kTABc_d", [3, 128, L], BF16, N)
        VABc_d = _dt(K, "VABc_d", [L, 390], BF16, N)
        oT_d = _dt(K, "oT_d", [16, 64, NT], BF16, N)
        x1_d = _dt(K, "x1_d", [NT, D], F32, N)
        u2T_d = _dt(K, "u2T_d", [8, 128, NT], BF16, N)
        gT_d = _dt(K, "gT_d", [16, NT], F32, N)
        rd = K.r_dram
        load_consts(K)
        nchunk = 8
        rows = NT // nchunk
        for i in range(nchunk):
            lo = i * rows
            hi = NT if i == nchunk - 1 else (i + 1) * rows
            _dma(K, x_d[lo:hi, :], x_in[lo:hi, :], [], [rd["x_d"]], f"i{i % 2}")
        A.push()
        zt = A.alloc((3 * HALO,), BF16)
        r_zt = P.res()
        P.add("dve", lambda h: h.memset(zt, 0.0), [], [r_zt])
        for off in (0, HALO + T):
            _dma(K, kTAB_d[:, :, off:off + HALO].rearrange("c p n -> p c n"), zt.rearrange("p (c n) -> p c n", c=3), [r_zt], [rd["kTAB_d"]], "z0")
            for j in range(HALO // 128):
                _dma(K, VAB_d[off + j * 128:off + (j + 1) * 128, :], zt[:, 0:390], [r_zt], [rd["VAB_d"]], "z1")
        P.barrier()
        A.pop()
        for l in range(nlayers):
            phase_mod(K, w_ada[l], b_ada2[l], cond, mod_d[l])

        def kdst(idx, tok0, nt, is_ctx):
            if idx == 3:
                return kTC_d[:, tok0:tok0 + nt]
            if is_ctx:
                return kTABc_d[idx][:, 0:nt]
            return kTAB_d[idx][:, HALO + tok0:HALO + tok0 + nt]

        def vdst(tok, is_ctx):
            if is_ctx:
                return [(VABc_d[tok - T:tok - T + 128, :], 0, 6), (VC_d[tok:tok + 128, :], 6, 8)]
            return [(VAB_d[HALO + tok:HALO + tok + 128, :], 0, 6), (VC_d[tok:tok + 128, :], 6, 8)]

        for l in range(nlayers):
            phase_qkv(K, x_d, mod_d[l], w_in_p[l], gains[l], ropeC, ropeS, pm_f, bd_f, qT_d, kdst, vdst)
            phase_attn(K, qT_d, kTC_d, VC_d, kTAB_d, VAB_d, kTABc_d, VABc_d, MA_d, MB_d[l], sink_b[l], oT_d)
            phase_proj(K, x_d, mod_d[l], oT_d, w_out_p[l], lnv[l], w_router, rbias, x1_d, u2T_d, gT_d)
            phase_moe(K, x1_d, mod_d[l], u2T_d, gT_d, [w1[l][e] for e in range(NEXP)], [w3[l][e] for e in range(NEXP)],
                      [w2[l][e] for e in range(NEXP)], lnv[l], sel_d, x_d)
        rows = T // nchunk
        for i in range(nchunk):
            _dma(K, x_out[i * rows:(i + 1) * rows, :], x_d[i * rows:(i + 1) * rows, :], [rd["x_d"]], [rd["x_out"]], f"i{i % 2}")
        P.barrier()
        P.emit(st)
        K.stats = (K.A.peak, K.P.n_sems, {e: len(K.P.ops[e]) for e in ENGS})
    return nc, K.stats


def _perm_w_in():
    def cols(base, h):
        return list(range(base + 64 * h, base + 64 * (h + 1)))
    p = []
    for a, b in ((0, 3), (1, 4), (2, 5)):
        p += cols(0, a) + cols(0, b)
    for a, b in ((0, 1), (2, 3)):
        p += cols(640, a) + cols(640, b)
    for a, b in ((0, 3), (1, 4), (2, 5)):
        p += cols(1408, a) + cols(1408, b)
    p += cols(384, 0) + cols(384, 1)
    p += cols(896, 0) + cols(896, 1) + cols(896, 2) + cols(896, 3)
    p += cols(1792, 0) + cols(1792, 1)
    p += cols(512, 0) + cols(512, 1)
    for h in range(4):
        p += cols(1152, h)
    p += cols(1920, 0) + cols(1920, 1)
    return np.array(p)


def _perm_w_out():
    rows = []
    for h in (0, 3, 1, 4, 2, 5):
        rows.append(np.arange(h * 64, (h + 1) * 64))
    for h in range(4):
        rows.append(np.arange(384 + h * 64, 384 + (h + 1) * 64))
    for h in (0, 3, 1, 4, 2, 5):
        rows.append(np.arange(640 + h * 64, 640 + (h + 1) * 64))
    return np.stack(rows)


def _rope_tables(cfg, q):
    t = np.arange(q * cfg.T, (q + 1) * cfg.T)
    row = (t // GRID_W).astype(np.float32)
    col = (t % GRID_W).astype(np.float32)
    axis_dim = HD // 2
    inv_freq = (np.float32(10000.0) ** (-np.arange(0, axis_dim, 2, dtype=np.float32) / np.float32(axis_dim))).astype(np.float32)
    ang = np.concatenate([row[:, None] * inv_freq, col[:, None] * inv_freq], -1).astype(np.float32)
    cos, sin = np.cos(ang).astype(np.float32), np.sin(ang).astype(np.float32)
    p = np.arange(128)
    d = p % 64
    pair = d // 2
    C = cos[:, pair].T.copy()
    sgn = np.where(d % 2 == 0, -1.0, 1.0).astype(np.float32)
    S = (sin[:, pair].T * sgn[:, None]).astype(np.float32)
    return np.ascontiguousarray(C), np.ascontiguousarray(S)


def _mask_A(cfg, q):
    M = np.full((cfg.NQT, 128, 3, 512), NEG, np.float32)
    ar = np.arange(128)
    for t in range(cfg.NQT):
        for b in range(4):
            gi = q * cfg.NQB + 4 * t + b
            qpos = gi * 128 + ar
            for oi, off in enumerate((-1, 0, 1)):
                gj = gi + off
                if gj < 0 or gj >= cfg.NBLK:
                    continue
                kpos = gj * 128 + ar
                ok = np.abs(qpos[None, :] - kpos[:, None]) <= 128
                M[t, :, oi, b * 128:(b + 1) * 128] = np.where(ok, 0.0, NEG)
    return M


def _mask_B(cfg, q, rpb_l):
    rows = cfg.S // GRID_W
    kh, kw = min(NA_ROWS, rows), NA_COLS
    M = np.full((4, 5, 128, 7, 128), NEG, np.float32)
    ar = np.arange(128)
    reps = {0: 0, 1: 1, 2: 2, 3: cfg.NQB - 2, 4: cfg.NQB - 1}
    for cl, lb in reps.items():
        if cl == 2 and cfg.NQB <= 4:
            continue
        gi = q * cfg.NQB + lb
        qpos = gi * 128 + ar
        r = qpos // GRID_W
        cq = qpos % GRID_W
        r0 = np.clip(r - kh // 2, 0, rows - kh)
        c0 = np.clip(cq - kw // 2, 0, GRID_W - kw)
        for oi, off in enumerate(range(-3, 4)):
            gj = gi + off
            if gj < 0 or gj >= cfg.NBLK:
                continue
            kpos = gj * 128 + ar
            kr = (kpos // GRID_W)[:, None]
            kc = (kpos % GRID_W)[:, None]
            ok = (kr >= r0[None]) & (kr < r0[None] + kh) & (kc >= c0[None]) & (kc < c0[None] + kw)
            ri = np.clip(kr - r[None] + NA_ROWS - 1, 0, 2 * NA_ROWS - 2)
            ci = np.clip(kc - cq[None] + NA_COLS - 1, 0, 2 * NA_COLS - 2)
            for h in range(4):
                M[h, cl, :, oi, :] = np.where(ok, rpb_l[h][ri, ci], NEG)
    return M


def _consts():
    ident = np.eye(128, dtype=np.float32)
    p = np.arange(128)
    pm = (p[:, None] == (p[None, :] ^ 1)).astype(np.float32)
    bd = ((p[:, None] // 64) == (p[None, :] // 64)).astype(np.float32)
    sel = np.zeros((16, 16, 128), np.float32)
    for e in range(16):
        sel[e, e, :] = 1.0
    return ident, pm, bd, sel.reshape(16, 16 * 128)


def host_inputs(cfg, inputs):
    ident, pm, bd, sel = _consts()
    perm = _perm_w_in()
    f = lambda a: np.ascontiguousarray(a, dtype=np.float32)
    w_in_p = f(inputs["w_in"][:, :, perm])
    w_out_p = f(inputs["w_out"][:, _perm_w_out()])
    lnv = f(np.stack([inputs["ln1_g"], inputs["ln1_b"], inputs["ln2_g"], inputs["ln2_b"]], 1))
    b_ada2 = f(np.stack([inputs["b_ada"]] * 2, 1))
    gains = f(np.stack([np.tile(inputs["q_gain"], (1, 2)), np.tile(inputs["k_gain"], (1, 2))], -1))
    sink_b = f(np.broadcast_to(inputs["sink"][:, None, :], (DEPTH, 128, 6)))
    C, S_ = _rope_tables(cfg, 0)
    MA = _mask_A(cfg, 0)
    MB = np.stack([_mask_B(cfg, 0, inputs["rpb"][l]) for l in range(DEPTH)])
    shared = {
        "w_ada": f(inputs["w_ada"]), "b_ada2": b_ada2, "w_in_p": w_in_p, "gains": gains,
        "ropeC": C, "ropeS": S_, "ident_f": ident, "pm_f": pm, "bd_f": bd,
        "MA_d": MA, "MB_d": MB, "sink_b": sink_b, "w_out_p": w_out_p, "lnv": lnv,
        "w_router": f(inputs["w_router"]), "rbias": f(inputs["router_bias"][None, :]), "sel": sel,
        "w1": f(inputs["w1"]), "w3": f(inputs["w3"]), "w2": f(inputs["w2"]),
    }
    maps = []
    for b in range(inputs["x"].shape[0]):
        m = dict(shared)
        m["x_in"] = f(np.concatenate([inputs["x"][b], inputs["ctx"][b]], 0))
        m["cond"] = f(np.stack([inputs["c"][b].reshape(8, 128).T, inputs["c_ctx"].reshape(8, 128).T], -1))
        maps.append(m)
    return maps


def build_qkv(cfg):
    nc = bass.Bass("TRN2", target_bir_lowering=False)
    with contextlib.ExitStack() as st:
        K = _new_ctx(nc, cfg, st)
        I, O = "ExternalInput", "ExternalOutput"
        x_d = _dt(K, "x_in", [cfg.NT, D], F32, I)
        cond = _dt(K, "cond", [128, 8, 2], F32, I)
        w_ada = _dt(K, "w_ada", [D, 6 * D], F32, I)
        b_ada2 = _dt(K, "b_ada2", [2, 6 * D], F32, I)
        w_in_p = _dt(K, "w_in_p", [D, 2048], F32, I)
        gains = _dt(K, "gains", [128, 2], F32, I)
        ropeC = _dt(K, "ropeC", [128, cfg.T], F32, I)
        ropeS = _dt(K, "ropeS", [128, cfg.T], F32, I)
        _dt(K, "ident_f", [128, 128], F32, I)
        pm_f = _dt(K, "pm_f", [128, 128], F32, I)
        bd_f = _dt(K, "bd_f", [128, 128], F32, I)
        mod_d = _dt(K, "mod_d", [2, 6 * D], F32, O)
        qT_d = _dt(K, "qT_d", [8, 128, cfg.NT], BF16, O)
        kT_d = _dt(K, "kT_d", [4, 128, cfg.NT], BF16, O)
        V_d = _dt(K, "V_d", [cfg.NT, 520], BF16, O)
        load_consts(K)
        phase_mod(K, w_ada, b_ada2, cond, mod_d)
        phase_qkv(K, x_d, mod_d, w_in_p, gains, ropeC, ropeS, pm_f, bd_f, qT_d,
                  lambda idx, tok0, nt, is_ctx: kT_d[idx][:, tok0:tok0 + nt],
                  lambda tok, is_ctx: [(V_d[tok:tok + 128, :], 0, 8)])
        K.P.emit(st)
        K.stats = (K.A.peak, K.P.n_sems, {e: len(K.P.ops[e]) for e in ENGS})
    return nc, K.stats


def build_rest(cfg):
    nc = bass.Bass("TRN2", target_bir_lowering=False)
    with contextlib.ExitStack() as st:
        K = _new_ctx(nc, cfg, st)
        I, O, N = "ExternalInput", "ExternalOutput", "Internal"
        x_d = _dt(K, "x_in", [cfg.NT, D], F32, I)
        mod_d = _dt(K, "mod_d", [2, 6 * D], F32, I)
        qT_d = _dt(K, "qT_d", [8, 128, cfg.NT], BF16, I)
        kTC_d = _dt(K, "kTC_d", [128, cfg.SA], BF16, I)
        VC_d = _dt(K, "VC_d", [cfg.SA, 130], BF16, I)
        kTAB_d = _dt(K, "kTAB_d", [3, 128, cfg.EXT], BF16, I)
        VAB_d = _dt(K, "VAB_d", [cfg.EXT, 390], BF16, I)
        kTABc_d = _dt(K, "kTABc_d", [3, 128, L], BF16, I)
        VABc_d = _dt(K, "VABc_d", [L, 390], BF16, I)
        MA_d = _dt(K, "MA_d", [cfg.NQT, 128, 3, 512], F32, I)
        MB_d = _dt(K, "MB_d", [4, 5, 128, 7, 128], F32, I)
        sink_b = _dt(K, "sink_b", [128, 6], F32, I)
        w_out_p = _dt(K, "w_out_p", [16, 64, D], F32, I)
        lnv = _dt(K, "lnv", [4, D], F32, I)
        w_router = _dt(K, "w_router", [D, NEXP], F32, I)
        rbias = _dt(K, "rbias", [1, NEXP], F32, I)
        sel_d = _dt(K, "sel", [16, 16 * 128], F32, I)
        w1 = _dt(K, "w1", [NEXP, D, DEXP], F32, I)
        w3 = _dt(K, "w3", [NEXP, D, DEXP], F32, I)
        w2 = _dt(K, "w2", [NEXP, DEXP, D], F32, I)
        _dt(K, "ident_f", [128, 128], F32, I)
        oT_d = _dt(K, "oT_d", [16, 64, cfg.NT], BF16, N)
        x1_d = _dt(K, "x1_d", [cfg.NT, D], F32, N)
        u2T_d = _dt(K, "u2T_d", [8, 128, cfg.NT], BF16, N)
        gT_d = _dt(K, "gT_d", [16, cfg.NT], F32, N)
        xo_d = _dt(K, "x_out", [cfg.NT, D], F32, O)
        load_consts(K)
        phase_attn(K, qT_d, kTC_d, VC_d, kTAB_d, VAB_d, kTABc_d, VABc_d, MA_d, MB_d, sink_b, oT_d)
        phase_proj(K, x_d, mod_d, oT_d, w_out_p, lnv, w_router, rbias, x1_d, u2T_d, gT_d)
        phase_moe(K, x1_d, mod_d, u2T_d, gT_d, [w1[e] for e in range(NEXP)], [w3[e] for e in range(NEXP)],
                  [w2[e] for e in range(NEXP)], lnv, sel_d, xo_d)
        K.P.emit(st)
        K.stats = (K.A.peak, K.P.n_sems, {e: len(K.P.ops[e]) for e in ENGS})
    return nc, K.stats


def host_qkv_inputs(cfg, inputs, l, x_cur, h_ctx):
    ident, pm, bd, _ = _consts()
    perm = _perm_w_in()
    w_in_p = np.ascontiguousarray(inputs["w_in"][l][:, perm])
    b_ada2 = np.ascontiguousarray(np.stack([inputs["b_ada"][l]] * 2))
    gains = np.ascontiguousarray(np.stack([np.tile(inputs["q_gain"][l], 2), np.tile(inputs["k_gain"][l], 2)], -1), np.float32)
    maps = []
    for core in range(8):
        b, q = core // 4, core % 4
        x_in = np.concatenate([x_cur[b, q * cfg.T:(q + 1) * cfg.T], h_ctx[b]], 0)
        cond = np.stack([inputs["c"][b].reshape(8, 128).T, inputs["c_ctx"].reshape(8, 128).T], -1)
        C, S_ = _rope_tables(cfg, q)
        maps.append({
            "x_in": np.ascontiguousarray(x_in, np.float32),
            "cond": np.ascontiguousarray(cond, np.float32),
            "w_ada": inputs["w_ada"][l], "b_ada2": b_ada2, "w_in_p": w_in_p, "gains": gains,
            "ropeC": C, "ropeS": S_, "ident_f": ident, "pm_f": pm, "bd_f": bd,
        })
    return maps


def host_rest_inputs(cfg, inputs, l, x_cur, h_ctx, qres):
    ident, pm, bd, sel = _consts()
    T = cfg.T
    w_out_p = np.ascontiguousarray(inputs["w_out"][l][_perm_w_out()])
    lnv = np.ascontiguousarray(np.stack([inputs["ln1_g"][l], inputs["ln1_b"][l], inputs["ln2_g"][l], inputs["ln2_b"][l]]))
    sink_b = np.ascontiguousarray(np.broadcast_to(inputs["sink"][l][None, :], (128, 6)), np.float32)
    rbias = np.ascontiguousarray(inputs["router_bias"][None, :])
    maps = []
    for core in range(8):
        b, q = core // 4, core % 4
        cores_b = [b * 4 + i for i in range(4)]
        kT = [np.asarray(qres[c]["kT_d"]) for c in cores_b]
        V = [np.asarray(qres[c]["V_d"]) for c in cores_b]
        kTC = np.concatenate([k[3][:, :T] for k in kT] + [kT[q][3][:, T:]], 1)
        VC = np.concatenate([v[:T, 390:520] for v in V] + [V[q][T:, 390:520]], 0)
        kfull = np.concatenate([k[0:3][:, :, :T] for k in kT], 2)
        kfull = np.pad(kfull, ((0, 0), (0, 0), (HALO, HALO)))
        kTAB = kfull[:, :, q * T:q * T + cfg.EXT]
        vfull = np.concatenate([v[:T, 0:390] for v in V], 0)
        vfull = np.pad(vfull, ((HALO, HALO), (0, 0)))
        VAB = vfull[q * T:q * T + cfg.EXT]
        x_in = np.concatenate([x_cur[b, q * T:(q + 1) * T], h_ctx[b]], 0)
        maps.append({
            "x_in": np.ascontiguousarray(x_in, np.float32),
            "mod_d": np.asarray(qres[core]["mod_d"]),
            "qT_d": np.asarray(qres[core]["qT_d"]),
            "kTC_d": np.ascontiguousarray(kTC), "VC_d": np.ascontiguousarray(VC),
            "kTAB_d": np.ascontiguousarray(kTAB), "VAB_d": np.ascontiguousarray(VAB),
            "kTABc_d": np.ascontiguousarray(kT[q][0:3][:, :, T:]), "VABc_d": np.ascontiguousarray(V[q][T:, 0:390]),
            "MA_d": _mask_A(cfg, q), "MB_d": _mask_B(cfg, q, inputs["rpb"][l]),
            "sink_b": sink_b, "w_out_p": w_out_p, "lnv": lnv,
            "w_router": inputs["w_router"], "rbias": rbias, "sel": sel,
            "w1": inputs["w1"][l], "w3": inputs["w3"][l], "w2": inputs["w2"][l],
            "ident_f": ident,
        })
    return maps


_PROGS = {}


def _get_prog(kind, cfg):
    key = (kind, cfg.S, cfg.NQ)
    if key not in _PROGS:
        _PROGS[key] = (build_qkv(cfg) if kind == "qkv" else build_rest(cfg))[0]
    return _PROGS[key]


def kernel(**inputs):
    inputs = {k: np.asarray(v) for k, v in inputs.items()}
    S = inputs["x"].shape[1]
    cfg = Cfg(S, NQ=4)
    x_cur = np.array(inputs["x"], dtype=np.float32)
    h_ctx = np.array(inputs["ctx"], dtype=np.float32)
    cores = list(range(8))
    for l in range(DEPTH):
        qres = run_bass_kernel_spmd(_get_prog("qkv", cfg), host_qkv_inputs(cfg, inputs, l, x_cur, h_ctx), core_ids=cores).results
        rres = run_bass_kernel_spmd(_get_prog("rest", cfg), host_rest_inputs(cfg, inputs, l, x_cur, h_ctx, qres), core_ids=cores).results
        x_new = np.empty_like(x_cur)
        h_new = np.empty_like(h_ctx)
        for core in cores:
            b, q = core // 4, core % 4
            xo = np.asarray(rres[core]["x_out"])
            x_new[b, q * cfg.T:(q + 1) * cfg.T] = xo[:cfg.T]
            if q == 0:
                h_new[b] = xo[cfg.T:]
        x_cur, h_ctx = x_new, h_new
    return x_cur
```
